# Optimizing a Trainium2 kernel written in Bass

```python
import jax, jax.numpy as jnp
from jax import lax
import numpy as np

D_MODEL = 1024
BATCH = 32
SEQ = 2048
DEPTH = 1

D_MIX = D_MODEL
HEAD_DIM = 64
ATTN_WIDTH = D_MIX // 2
N_Q_HEADS = ATTN_WIDTH // HEAD_DIM
N_KV_HEADS = N_Q_HEADS // 4
KV_WIDTH = N_KV_HEADS * HEAD_DIM
CONV_WIDTH = D_MIX - ATTN_WIDTH
CONV_GROUPS = CONV_WIDTH // HEAD_DIM
CONV_KERNEL = 31
WINDOW = 128
BLOCK = 128
ROPE_THETA = 500000.0
ROT_DIM = HEAD_DIM // 4
Q_END = ATTN_WIDTH
K_END = Q_END + KV_WIDTH
V_END = K_END + KV_WIDTH
CA_END = V_END + CONV_WIDTH
IN_COLS = CA_END + CONV_WIDTH
N_GROUPS = 4
EXPERTS_PER_GROUP = 8
N_EXPERTS = N_GROUPS * EXPERTS_PER_GROUP
TOP_K = 2
D_EXPERT = D_MODEL // 2
EXPERT_BLOCK = 512
EPS = 1e-5

kernel_name = 'hymba_conformer_swa_sink_hmoe_block'


def rms_norm(x, w):
    xf = x.astype(jnp.float32)
    y = xf * lax.rsqrt(jnp.mean(xf * xf, axis=-1, keepdims=True) + EPS)
    return (y * w.astype(jnp.float32)).astype(x.dtype)


def layer_norm(x, w, b):
    xf = x.astype(jnp.float32)
    mu = jnp.mean(xf, axis=-1, keepdims=True)
    xc = xf - mu
    y = xc * lax.rsqrt(jnp.mean(xc * xc, axis=-1, keepdims=True) + EPS)
    return (y * w.astype(jnp.float32) + b.astype(jnp.float32)).astype(x.dtype)


def partial_rope(t, positions):
    half = ROT_DIM // 2
    inv_freq = jnp.power(ROPE_THETA, -jnp.arange(half, dtype=jnp.float32) * 2.0 / ROT_DIM)
    ang = positions.astype(jnp.float32)[..., None] * inv_freq
    cos = jnp.cos(ang)[:, :, None, :]
    sin = jnp.sin(ang)[:, :, None, :]
    tf = t.astype(jnp.float32)
    x1 = tf[..., :half]
    x2 = tf[..., half:ROT_DIM]
    out = jnp.concatenate([x1 * cos - x2 * sin, x2 * cos + x1 * sin, tf[..., ROT_DIM:]], axis=-1)
    return out.astype(t.dtype)


def sliding_window_sink_attention(q, k, v, sinks):
    B, S = q.shape[0], q.shape[1]
    nb = S // BLOCK
    G = N_Q_HEADS // N_KV_HEADS
    qb = q.reshape(B, nb, BLOCK, N_KV_HEADS, G, HEAD_DIM)
    kb = k.reshape(B, nb, BLOCK, N_KV_HEADS, HEAD_DIM)
    vb = v.reshape(B, nb, BLOCK, N_KV_HEADS, HEAD_DIM)

    def with_prev(t):
        prev = jnp.pad(t[:, :-1], ((0, 0), (1, 0), (0, 0), (0, 0), (0, 0)))
        return jnp.concatenate([prev, t], axis=2)

    kk = with_prev(kb)
    vv = with_prev(vb)
    s = jnp.einsum('bnqhgd,bnkhd->bnhgqk', qb.astype(jnp.float32), kk.astype(jnp.float32))
    s = s * (HEAD_DIM ** -0.5)
    qi = jnp.arange(BLOCK)[:, None]
    kj = jnp.arange(2 * BLOCK)[None, :]
    dist = qi + BLOCK - kj
    band = (dist >= 0) & (dist < WINDOW)
    valid = jnp.where((jnp.arange(nb) == 0)[:, None, None], band & (kj >= BLOCK), band)
    s = jnp.where(valid[None, :, None, None], s, -jnp.inf)
    sink = jnp.broadcast_to(sinks.astype(jnp.float32).reshape(N_KV_HEADS, G)[None, None, :, :, None, None],
                            s.shape[:-1] + (1,))
    p = jax.nn.softmax(jnp.concatenate([s, sink], axis=-1), axis=-1)[..., :-1]
    o = jnp.einsum('bnhgqk,bnkhd->bnqhgd', p.astype(v.dtype), vv)
    return o.reshape(B, S, N_Q_HEADS * HEAD_DIM)


def conformer_conv(a, gate, dw_w, dw_b, ln_w, ln_b):
    u = a * jax.nn.sigmoid(gate)
    u = lax.conv_general_dilated(u, dw_w[:, None, :], window_strides=(1,),
                                 padding=[(CONV_KERNEL - 1, 0)],
                                 dimension_numbers=('NWC', 'WIO', 'NWC'),
                                 feature_group_count=CONV_WIDTH) + dw_b
    u = layer_norm(u, ln_w, ln_b)
    return jax.nn.silu(u)


def routed_swiglu_experts(xf, expert_idx, gates, w_gate, w_up, w_down):
    N, D = xf.shape
    n_slots = N * TOP_K
    nb = -(-n_slots // EXPERT_BLOCK) + N_EXPERTS
    flat_e = expert_idx.reshape(-1)
    order = jnp.argsort(flat_e)
    sorted_e = flat_e[order]
    tok = (order // TOP_K).astype(jnp.int32)
    counts = jnp.bincount(flat_e, length=N_EXPERTS)
    padded = ((counts + EXPERT_BLOCK - 1) // EXPERT_BLOCK) * EXPERT_BLOCK
    cum_padded = jnp.cumsum(padded)
    pstart = cum_padded - padded
    start = jnp.cumsum(counts) - counts
    rank = jnp.arange(n_slots) - start[sorted_e]
    dest = pstart[sorted_e] + rank
    row_tok = jnp.full((nb * EXPERT_BLOCK,), N, jnp.int32).at[dest].set(tok)
    row_gate = jnp.zeros((nb * EXPERT_BLOCK,), xf.dtype).at[dest].set(gates.reshape(-1)[order])
    block_expert = jnp.minimum(
        jnp.searchsorted(cum_padded, jnp.arange(nb) * EXPERT_BLOCK, side='right'), N_EXPERTS - 1)
    xpad = jnp.concatenate([xf, jnp.zeros((1, D), xf.dtype)], axis=0)

    def expert_block(args):
        rows, e = args
        xb = xpad[rows]
        hb = jax.nn.silu(xb @ w_gate[e]) * (xb @ w_up[e])
        return hb @ w_down[e]

    yb = lax.map(expert_block, (row_tok.reshape(nb, EXPERT_BLOCK), block_expert))
    y = jax.ops.segment_sum(yb.reshape(-1, D) * row_gate[:, None], row_tok, num_segments=N + 1)
    return y[:N]


def hierarchical_moe(h, rg_w, rg_b, re_w, re_b, w_gate, w_up, w_down):
    B, S, D = h.shape
    xf = h.reshape(B * S, D)
    g_logits = (xf @ rg_w).astype(jnp.float32) + rg_b.astype(jnp.float32)
    g_prob = jax.nn.softmax(g_logits, axis=-1)
    g_idx = jnp.argmax(g_logits, axis=-1)
    g_p = jnp.take_along_axis(g_prob, g_idx[:, None], axis=1)[:, 0]
    e_logits = ((xf @ re_w).astype(jnp.float32) + re_b.astype(jnp.float32))
    e_logits = e_logits.reshape(-1, N_GROUPS, EXPERTS_PER_GROUP)
    e_logits = jnp.take_along_axis(e_logits, g_idx[:, None, None], axis=1)[:, 0]
    e_prob = jax.nn.softmax(e_logits, axis=-1)
    top_p, top_i = lax.top_k(e_prob, TOP_K)
    gates = (g_p[:, None] * top_p / jnp.sum(top_p, axis=-1, keepdims=True)).astype(h.dtype)
    expert_idx = (g_idx[:, None] * EXPERTS_PER_GROUP + top_i).astype(jnp.int32)
    y = routed_swiglu_experts(xf, expert_idx, gates, w_gate, w_up, w_down)
    return y.reshape(B, S, D)


def setup_inputs(seed: int = 0) -> dict:
    key = jax.random.key(seed)
    ks = jax.random.split(key, 24)
    f32 = jnp.float32

    def nrm(k, shape, scale):
        return jax.random.normal(k, shape, f32) * scale

    def gain(k, shape):
        return 1.0 + 0.02 * jax.random.normal(k, shape, f32)

    L = DEPTH
    return {
        'x': nrm(ks[0], (BATCH, SEQ, D_MODEL), 1.0),
        'positions': jnp.broadcast_to(jnp.arange(SEQ, dtype=jnp.int32), (BATCH, SEQ)),
        'attn_norm_w': gain(ks[1], (L, D_MODEL)),
        'w_in': nrm(ks[2], (L, D_MODEL, IN_COLS), D_MODEL ** -0.5),
        'attn_sinks': nrm(ks[3], (L, N_Q_HEADS), 0.5),
        'conv_dw_w': nrm(ks[4], (L, CONV_KERNEL, CONV_WIDTH), CONV_KERNEL ** -0.5),
        'conv_dw_b': nrm(ks[5], (L, CONV_WIDTH), 0.02),
        'conv_ln_w': gain(ks[6], (L, CONV_WIDTH)),
        'conv_ln_b': nrm(ks[7], (L, CONV_WIDTH), 0.02),
        'attn_out_norm_w': gain(ks[8], (L, ATTN_WIDTH)),
        'conv_out_norm_w': gain(ks[9], (L, CONV_WIDTH)),
        'w_out': nrm(ks[10], (L, D_MIX, D_MODEL), D_MIX ** -0.5),
        'ffn_norm_w': gain(ks[11], (L, D_MODEL)),
        'router_group_w': nrm(ks[12], (L, D_MODEL, N_GROUPS), D_MODEL ** -0.5),
        'router_group_b': nrm(ks[13], (L, N_GROUPS), 0.01),
        'router_expert_w': nrm(ks[14], (L, D_MODEL, N_EXPERTS), D_MODEL ** -0.5),
        'router_expert_b': nrm(ks[15], (L, N_EXPERTS), 0.01),
        'w_gate': nrm(ks[16], (L, N_EXPERTS, D_MODEL, D_EXPERT), D_MODEL ** -0.5),
        'w_up': nrm(ks[17], (L, N_EXPERTS, D_MODEL, D_EXPERT), D_MODEL ** -0.5),
        'w_down': nrm(ks[18], (L, N_EXPERTS, D_EXPERT, D_MODEL), D_EXPERT ** -0.5),
        'final_norm_w': gain(ks[19], (D_MODEL,)),
    }


def reference(x, positions, attn_norm_w, w_in, attn_sinks, conv_dw_w, conv_dw_b, conv_ln_w,
              conv_ln_b, attn_out_norm_w, conv_out_norm_w, w_out, ffn_norm_w, router_group_w,
              router_group_b, router_expert_w, router_expert_b, w_gate, w_up, w_down,
              final_norm_w):
    B, S = x.shape[0], x.shape[1]
    for l in range(DEPTH):
        hn = rms_norm(x, attn_norm_w[l])
        proj = hn @ w_in[l]
        q = proj[..., :Q_END].reshape(B, S, N_Q_HEADS, HEAD_DIM)
        k = proj[..., Q_END:K_END].reshape(B, S, N_KV_HEADS, HEAD_DIM)
        v = proj[..., K_END:V_END].reshape(B, S, N_KV_HEADS, HEAD_DIM)
        conv_a = proj[..., V_END:CA_END]
        conv_g = proj[..., CA_END:]
        q = partial_rope(q, positions)
        k = partial_rope(k, positions)
        attn = sliding_window_sink_attention(q, k, v, attn_sinks[l])
        conv = conformer_conv(conv_a, conv_g, conv_dw_w[l], conv_dw_b[l],
                              conv_ln_w[l], conv_ln_b[l])
        mixed = jnp.concatenate([rms_norm(attn, attn_out_norm_w[l]),
                                 rms_norm(conv, conv_out_norm_w[l])], axis=-1)
        x = x + mixed @ w_out[l]
        hf = rms_norm(x, ffn_norm_w[l])
        x = x + hierarchical_moe(hf, router_group_w[l], router_group_b[l], router_expert_w[l],
                                 router_expert_b[l], w_gate[l], w_up[l], w_down[l])
    return rms_norm(x, final_norm_w)
```

```python
import os
import numpy as np
import ml_dtypes
from contextlib import ExitStack

import concourse.bass as bass
import concourse.mybir as mybir
from concourse.bass_utils import run_bass_kernel_spmd

F32 = mybir.dt.float32
BF16 = mybir.dt.bfloat16
I32 = mybir.dt.int32
AF = mybir.ActivationFunctionType
ALU = mybir.AluOpType
AX = mybir.AxisListType

N_CORES = 8
SEQ = 2048
D = 1024
NCOL = 1792
NE = 32
DE = 512
NT_SEQ = SEQ // 128
GT = 2
GW = GT * 128
HALO = 30
KTAPS = 31
BLK = 512
EPS = 1e-5
ROT = 30000
NDMASEM = 40
THETA = 500000.0
NEG = -30000.0

VF = 161
VB = 1024 + 36 + 8 + 8 + 32 + 64


class _Stop(Exception):
    pass


class Res:
    __slots__ = ("name", "w", "r", "excl")

    def __init__(self, name, excl=False):
        self.name = name
        self.w = {}
        self.r = {}
        self.excl = excl


class Buf:
    def __init__(self, t, name):
        self.t = t
        self.res = Res(name)


def _res(x):
    return x.res if isinstance(x, Buf) else x


class Sched:
    def __init__(self, nc, es):
        self.nc = nc
        self.es = es
        self.eng = {"pe": nc.tensor, "act": nc.scalar, "dve": nc.vector,
                    "pool": nc.gpsimd, "sp": nc.sync}
        self.csem = {e: [] for e in ("pe", "act", "dve", "pool")}
        self.ccnt = {e: 0 for e in ("pe", "act", "dve", "pool")}
        self.waited = {}
        self.dsem = {"sp": [], "pool": []}
        self.dcnt = {"sp": [], "pool": []}
        self.drr = {"sp": 0, "pool": 0}
        self.dmax = {"sp": 28, "pool": 16}
        self.n_wait = 0
        self.n_ins = 0
        self.defer = None
        self.defer_dma = []

    def _cur_event(self, e):
        n = self.ccnt[e]
        idx = n // ROT
        while len(self.csem[e]) <= idx:
            self.csem[e].append(self.es.enter_context(
                self.nc.semaphore(f"c_{e}_{len(self.csem[e])}")))
        return (self.csem[e][idx], (n % ROT) + 1, e)

    def _wait(self, e, ev):
        sem, val, src = ev
        if src == "pe" and e == "pe":
            return
        key = (e, sem.name)
        if self.waited.get(key, 0) >= val:
            return
        if self.defer is not None and e in self.defer:
            self.defer[e].append(lambda: self.eng[e].wait_ge(sem, val))
        else:
            self.eng[e].wait_ge(sem, val)
        self.waited[key] = val
        self.n_wait += 1

    @staticmethod
    def _merge(d, ev):
        sem, val, src = ev
        old = d.get(sem.name)
        if old is None or old[1] < val:
            d[sem.name] = ev

    def _deps(self, e, reads, writes, multi):
        for r in reads:
            rs = _res(r)
            for ev in list(rs.w.values()):
                self._wait(e, ev)
            if rs.excl:
                for ev in list(rs.r.values()):
                    if ev[2] != e:
                        self._wait(e, ev)
        for w in list(writes) + list(multi):
            rs = _res(w)
            for ev in list(rs.r.values()):
                self._wait(e, ev)
        for w in writes:
            for ev in list(_res(w).w.values()):
                self._wait(e, ev)

    def _commit(self, ev, reads, writes, multi):
        for r in reads:
            self._merge(_res(r).r, ev)
        for w in writes:
            rs = _res(w)
            rs.w = {ev[0].name: ev}
            rs.r = {}
        for w in multi:
            rs = _res(w)
            self._merge(rs.w, ev)

    def op(self, e, fn, reads=(), writes=(), inc=True, multi=()):
        self._deps(e, reads, writes, multi)
        ev = self._cur_event(e)
        if self.defer is not None and e in self.defer:
            def emit(fn=fn, ev=ev, inc=inc, e=e):
                ins_ = fn(self.eng[e])
                if inc:
                    ins_.then_inc(ev[0], 1)
            self.defer[e].append(emit)
            ins = None
        else:
            ins = fn(self.eng[e])
            if inc:
                ins.then_inc(ev[0], 1)
        if inc:
            self.ccnt[e] += 1
        self._commit(ev, reads, writes, multi)
        self.n_ins += 1
        return ins

    def dma(self, q, fn, reads=(), writes=(), multi=()):
        self._deps(q, reads, writes, multi)
        dsem, dcnt = self.dsem[q], self.dcnt[q]
        if len(dsem) < self.dmax[q]:
            dsem.append(self.es.enter_context(self.nc.semaphore(f"d_{q}_{len(dsem)}")))
            dcnt.append(0)
            slot = len(dsem) - 1
        else:
            slot = self.drr[q] % self.dmax[q]
        self.drr[q] += 1
        sem = dsem[slot]
        if dcnt[slot] > 0:
            self._wait(q, (sem, dcnt[slot] * 16, "dma"))
        dcnt[slot] += 1
        ev = (sem, dcnt[slot] * 16, "dma")
        if self.defer is not None and q in self.defer:
            def emit(fn=fn, sem=sem, q=q):
                fn(self.eng[q]).then_inc(sem, 16)
            self.defer[q].append(emit)
            self.defer_dma.append((sem, (dcnt[slot] - 1) * 16))
            ins = None
        else:
            ins = fn(self.eng[q])
            ins.then_inc(sem, 16)
        self._commit(ev, reads, writes, multi)
        self.n_ins += 1
        return ins

    def barrier(self):
        evs = []
        for e in ("pe", "act", "dve", "pool"):
            n = self.ccnt[e]
            if n > 0:
                idx = (n - 1) // ROT
                evs.append((self.csem[e][idx], ((n - 1) % ROT) + 1, e))
        for q_ in ("sp", "pool"):
            for slot, sem in enumerate(self.dsem[q_]):
                if self.dcnt[q_][slot] > 0:
                    evs.append((sem, self.dcnt[q_][slot] * 16, "dma"))
        for q in ("pe", "act", "dve", "pool", "sp"):
            for ev in evs:
                sem, val, src = ev
                if src == q:
                    if q == "pe":
                        continue
                key = (q, sem.name)
                if self.waited.get(key, 0) >= val:
                    continue
                self.eng[q].wait_ge(sem, val)
                self.waited[key] = val
                self.n_wait += 1

    def wait_all(self, q, resources):
        for r in resources:
            rs = _res(r)
            for ev in list(rs.w.values()) + list(rs.r.values()):
                self._wait(q, ev)


def build_program(nseq, debug=False, stop=None):
    NT = nseq * NT_SEQ
    NG = NT // GT
    NTOK = NT * 128
    NBLK = (NTOK * 2) // BLK + NE
    NROWS = NBLK * BLK
    RT = BLK // 128

    nc = bass.Bass("TRN2", target_bir_lowering=False)
    dram_in = lambda n, s, dt: nc.dram_tensor(n, s, dt, kind="ExternalInput").ap()
    x_d = dram_in("x", [NTOK, D], F32)
    pos_d = dram_in("pos", [128, NT], I32)
    win_d = dram_in("w_in", [D, NCOL], F32)
    wout_d = dram_in("w_out", [D, D], F32)
    wr_d = dram_in("w_r", [D, 36], F32)
    wg_d = dram_in("w_gate", [NE, D, DE], F32)
    wu_d = dram_in("w_up", [NE, D, DE], F32)
    wd_d = dram_in("w_down", [NE, DE, D], F32)
    vfm_d = dram_in("vfm", [128, VF], F32)
    vbc_d = dram_in("vbc", [128, VB], F32)
    fnw_d = dram_in("fnw", [128, D], F32)
    cbf_d = dram_in("cbf", [128, 6 * 128], BF16)
    out_d = nc.dram_tensor("out", [NTOK, D], F32, kind="ExternalOutput").ap()
    x1_d = nc.dram_tensor("x1s", [NTOK, D], F32,
                          kind="ExternalOutput" if debug else "Internal").ap()
    xs_d = nc.dram_tensor("xs", [NROWS, D], BF16).ap()
    hf_d = nc.dram_tensor("hfd", [NTOK, D], BF16).ap()
    yb_d = nc.dram_tensor("yb", [NROWS, D], BF16).ap()
    if debug:
        dbg_d = nc.dram_tensor("dbg", [128, NT, 4], F32, kind="ExternalOutput").ap()

    with ExitStack() as es:
        S = Sched(nc, es)

        def sb(es_, name, shape, dt):
            return Buf(es_.enter_context(nc.sbuf_tensor("s_" + name, shape, dt)), name)

        def ps(es_, name, shape, dt):
            b = Buf(es_.enter_context(nc.psum_tensor("p_" + name, shape, dt)), name)
            b.res.excl = True
            return b

        V = lambda fn, r=(), w=(), **k: S.op("dve", fn, r, w, **k)
        A = lambda fn, r=(), w=(), **k: S.op("act", fn, r, w, **k)
        Gp = lambda fn, r=(), w=(), **k: S.op("pool", fn, r, w, **k)
        T = lambda fn, r=(), w=(), **k: S.op("pe", fn, r, w, **k)
        DM = lambda fn, r=(), w=(), **k: S.dma("sp", fn, r, w, **k)
        DG = lambda fn, r=(), w=(), **k: S.dma("pool", fn, r, w, **k)

        bc_reg = nc.gpsimd.alloc_register("bc_rows")
        nc.gpsimd.reg_mov(bc_reg, NROWS - 1)
        bcw_reg = nc.gpsimd.alloc_register("bc_wrows")
        nc.gpsimd.reg_mov(bcw_reg, NE * 128 - 1)

        vfm = sb(es, "vfm", [128, VF], F32)
        vbc = sb(es, "vbc", [128, VB], F32)
        cbf = sb(es, "cbf", [128, 6, 128], BF16)
        dest = sb(es, "dest", [128, NT, 2], I32)
        gates = sb(es, "gates", [128, NT, 2], F32)
        exf = sb(es, "exf", [128, NT, 2], F32)
        rkf = sb(es, "rkf", [128, NT, 2], F32)
        cnt = sb(es, "cnt", [128, 32], F32)
        widx = sb(es, "widx", [128, 64], I32)
        nused_i = sb(es, "nused_i", [128, 1], I32)
        hf_res = [Res(f"hfd{t}") for t in range(NT)]
        x1_res = [Res(f"x1d{t}") for t in range(NT)]
        xs_res = Res("xs")
        xs_zero = Res("xs_zero")
        yb_zero = Res("yb_zero")
        yb_res = Res("yb")

        DM(lambda e: e.dma_start(out=vfm.t[:], in_=vfm_d), [], [vfm])
        DM(lambda e: e.dma_start(out=vbc.t[:], in_=vbc_d), [], [vbc])
        DM(lambda e: e.dma_start(out=cbf.t[:].rearrange("p a b -> p (a b)"), in_=cbf_d), [], [cbf])
        ident = cbf.t[:, 0, :]
        Utri = cbf.t[:, 1, :]
        ones_b = cbf.t[:, 2, :]
        onesm = cbf.t[:, 3, :]
        maskcur = cbf.t[:, 4, :]
        maskprev = cbf.t[:, 5, :]
        ffw_bc = vbc.t[:, 0:1024]
        rbias = vbc.t[:, 1024:1060]
        sinks_bc = vbc.t[:, 1060:1068]
        invf_bc = vbc.t[:, 1068:1076]
        iota_bc = vbc.t[:, 1076:1108]
        bstart_bc = vbc.t[:, 1108:1172]

        with ExitStack() as esA:
            w_in_bf = sb(esA, "w_in_bf", [128, 8, NCOL], BF16)
            w_in_res = [Res(f"w_in{k}") for k in range(8)]
            w_out_bf = sb(esA, "w_out_bf", [128, 8, D], BF16)
            w_out_res = [Res(f"w_out{k}") for k in range(8)]
            wr_bf = sb(esA, "wr_bf", [128, 8, 36], BF16)
            diag = sb(esA, "diag", [128, 4, KTAPS, 128], BF16)
            diag_res = [Res(f"diag{c}") for c in range(4)]
            ropeC = sb(esA, "ropeC", [128, NT, 16], F32)
            ropeS = sb(esA, "ropeS", [128, NT, 16], F32)
            esink = sb(esA, "esink", [128, 8], F32)
            nln = sb(esA, "nln", [128, 8], F32)
            junk = esA.enter_context(nc.sbuf_tensor("s_junk", [128, 1024], BF16))

            tpb = [ps(esA, f"tpb{i}", [128, 1024], BF16) for i in range(2)]
            mm = [ps(esA, f"mm{i}", [128, 512], F32) for i in range(6)]
            st_ = {"mm": 0, "tp": 0}

            def next_mm():
                b = mm[st_["mm"] % 6]
                st_["mm"] += 1
                return b

            def next_tp():
                b = tpb[st_["tp"] % 2]
                st_["tp"] += 1
                return b

            with ExitStack() as es0:
                stg = [sb(es0, f"stg{i}", [128, NCOL], F32) for i in range(2)]
                ident_f = sb(es0, "ident_f", [128, 128], F32)
                posf = sb(es0, "posf", [128, NT], F32)
                posi = sb(es0, "posi", [128, NT], I32)
                ang = sb(es0, "ang", [128, NT, 16], F32)
                kf = sb(es0, "kf", [128, NT, 16], F32)
                ki = sb(es0, "ki", [128, NT, 16], I32)
                mk = sb(es0, "mk", [128, NT, 16], F32)
                trg = sb(es0, "trg", [128, NT, 16], F32)

                DM(lambda e: e.dma_start(out=posi.t[:], in_=pos_d), [], [posi])
                V(lambda e: e.tensor_copy(out=posf.t[:], in_=posi.t[:]), [posi], [posf])
                V(lambda e: e.tensor_tensor(out=ang.t[:, :, 0:8],
                                            in0=posf.t[:].unsqueeze(2).to_broadcast([128, NT, 8]),
                                            in1=invf_bc.unsqueeze(1).to_broadcast([128, NT, 8]),
                                            op=ALU.mult), [posf, vbc], [ang])
                V(lambda e: e.tensor_scalar(out=ang.t[:, :, 8:16], in0=ang.t[:, :, 0:8],
                                            scalar1=float(np.pi / 2), scalar2=None, op0=ALU.add),
                  [ang], [ang])
                V(lambda e: e.tensor_scalar(out=kf.t[:], in0=ang.t[:], scalar1=float(1.0 / (2 * np.pi)),
                                            scalar2=None, op0=ALU.mult), [ang], [kf])
                V(lambda e: e.tensor_copy(out=ki.t[:], in_=kf.t[:]), [kf], [ki])
                V(lambda e: e.tensor_copy(out=kf.t[:], in_=ki.t[:]), [ki], [kf])
                V(lambda e: e.scalar_tensor_tensor(out=ang.t[:], in0=kf.t[:], scalar=float(-2 * np.pi),
                                                   in1=ang.t[:], op0=ALU.mult, op1=ALU.add),
                  [kf, ang], [ang])
                V(lambda e: e.tensor_single_scalar(out=mk.t[:], in_=ang.t[:], scalar=float(np.pi),
                                                   op=ALU.is_gt), [ang], [mk])
                V(lambda e: e.scalar_tensor_tensor(out=ang.t[:], in0=mk.t[:], scalar=float(-2 * np.pi),
                                                   in1=ang.t[:], op0=ALU.mult, op1=ALU.add),
                  [mk, ang], [ang])
                V(lambda e: e.tensor_single_scalar(out=mk.t[:], in_=ang.t[:], scalar=float(-np.pi),
                                                   op=ALU.is_lt), [ang], [mk])
                V(lambda e: e.scalar_tensor_tensor(out=ang.t[:], in0=mk.t[:], scalar=float(2 * np.pi),
                                                   in1=ang.t[:], op0=ALU.mult, op1=ALU.add),
                  [mk, ang], [ang])
                V(lambda e: e.tensor_scalar(out=ang.t[:], in0=ang.t[:], scalar1=float(np.pi), scalar2=float(-np.pi),
                                            op0=ALU.min, op1=ALU.max), [ang], [ang])
                A(lambda e: e.activation(out=trg.t[:], in_=ang.t[:], func=AF.Sin), [ang], [trg])
                V(lambda e: e.tensor_copy(out=ropeC.t[:, :, 0:8], in_=trg.t[:, :, 8:16]), [trg], [ropeC])
                V(lambda e: e.tensor_copy(out=ropeC.t[:, :, 8:16], in_=trg.t[:, :, 8:16]), [trg], [ropeC])
                V(lambda e: e.tensor_scalar(out=ropeS.t[:, :, 0:8], in0=trg.t[:, :, 0:8], scalar1=-1.0,
                                            scalar2=None, op0=ALU.mult), [trg], [ropeS])
                V(lambda e: e.tensor_copy(out=ropeS.t[:, :, 8:16], in_=trg.t[:, :, 0:8]), [trg], [ropeS])

                A(lambda e: e.activation(out=esink.t[:], in_=sinks_bc, func=AF.Exp), [vbc], [esink])
                V(lambda e: e.tensor_scalar(out=nln.t[:], in0=vfm.t[:, 28:36], scalar1=-1.0, scalar2=None,
                                            op0=ALU.mult), [vfm], [nln])
                V(lambda e: e.memset(cnt.t[:], 0.0), [], [cnt])
                V(lambda e: e.tensor_copy(out=ident_f.t[:], in_=ident), [cbf], [ident_f])

                cast_engs = ["act", "dve", "act"]
                ci = 0
                for k in range(8):
                    s_ = stg[k % 2]
                    DM(lambda e, s_=s_, k=k: e.dma_start(out=s_.t[:], in_=win_d[k * 128:(k + 1) * 128, :]), [], [s_])
                    eng = cast_engs[ci % 3]; ci += 1
                    if eng == "act":
                        A(lambda e, s_=s_, k=k: e.activation(out=w_in_bf.t[:, k, :], in_=s_.t[:], func=AF.Copy,
                                                             scale=vfm.t[:, k:k + 1]), [s_, vfm], [w_in_res[k]])
                    else:
                        S.op(eng, lambda e, s_=s_, k=k: e.tensor_scalar(out=w_in_bf.t[:, k, :], in0=s_.t[:],
                                                                         scalar1=vfm.t[:, k:k + 1], scalar2=None,
                                                                         op0=ALU.mult), [s_, vfm], [w_in_res[k]])
                for k in range(8):
                    s_ = stg[k % 2]
                    DM(lambda e, s_=s_, k=k: e.dma_start(out=s_.t[:, 0:D], in_=wout_d[k * 128:(k + 1) * 128, :]), [], [s_])
                    eng = cast_engs[ci % 3]; ci += 1
                    if eng == "act":
                        A(lambda e, s_=s_, k=k: e.activation(out=w_out_bf.t[:, k, :], in_=s_.t[:, 0:D], func=AF.Copy,
                                                             scale=vfm.t[:, 16 + k:17 + k]), [s_, vfm], [w_out_res[k]])
                    else:
                        S.op(eng, lambda e, s_=s_, k=k: e.tensor_scalar(out=w_out_bf.t[:, k, :], in0=s_.t[:, 0:D],
                                                                         scalar1=vfm.t[:, 16 + k:17 + k], scalar2=None,
                                                                         op0=ALU.mult), [s_, vfm], [w_out_res[k]])
                s_ = stg[0]
                DM(lambda e: e.dma_start(out=stg[0].t[:, 0:8 * 36].rearrange("p (k c) -> p k c", k=8),
                                         in_=wr_d.rearrange("(k p) c -> p k c", p=128)), [], [stg[0]])
                V(lambda e: e.tensor_copy(out=wr_bf.t[:], in_=stg[0].t[:, 0:8 * 36].rearrange("p (k c) -> p k c", k=8)),
                  [stg[0]], [wr_bf])
                di = 0
                for c in range(4):
                    for j in range(KTAPS):
                        di += 1
                        col = 36 + c * KTAPS + j
                        if di % 2 == 0:
                            A(lambda e, c=c, j=j, col=col: e.activation(out=diag.t[:, c, j, :], in_=ident_f.t[:], func=AF.Copy,
                                                                         scale=vfm.t[:, col:col + 1]), [ident_f, vfm], [],
                              multi=[diag_res[c]])
                        else:
                            V(lambda e, c=c, j=j, col=col: e.tensor_scalar(
                                out=diag.t[:, c, j, :], in0=ident_f.t[:], scalar1=vfm.t[:, col:col + 1],
                                scalar2=None, op0=ALU.mult), [ident_f, vfm], [], multi=[diag_res[c]])
                S.barrier()

            substop = None
            if stop and stop.startswith('A') and ':' in stop:
                substop = int(stop.split(':')[1])
                stop = stop.split(':')[0]
            NGrun = NG if not (stop and stop.startswith('A')) or stop == 'A' else int(stop[1:])

            def chk(k_):
                if substop == k_:
                    raise _Stop()
            xsb = [[sb(esA, f"xsb{p}{j}", [128, D], F32) for j in range(GT)] for p in range(3)]
            ssx = sb(esA, "ssx", [128, GT], F32)
            lnx = sb(esA, "lnx", [128, GT], F32)
            rstx = sb(esA, "rstx", [128, GT], F32)
            xsbf = [sb(esA, f"xsbf{j}", [128, D], BF16) for j in range(GT)]
            hnT = [sb(esA, "hnT0", [128, 8, GW], BF16)] * 2
            qsb = [sb(esA, f"qsb{j}", [128, 512], BF16) for j in range(GT)]
            ksb = [sb(esA, f"ksb{j}", [128, 128], BF16) for j in range(GT)]
            rt1 = [sb(esA, f"rt1{j}", [128, 10, 16], F32) for j in range(GT)]
            rt2 = [sb(esA, f"rt2{j}", [128, 10, 16], F32) for j in range(GT)]
            qTs = [[sb(esA, f"qT{p}{j}", [128, 4, 128], BF16) for j in range(GT)] for p in range(2)]
            kTb = [sb(esA, f"kT{i}", [128, 128], BF16) for i in range(5)]
            vaug = [sb(esA, f"vaug{i}", [128, 2, 65], BF16) for i in range(5)]
            Esb = [sb(esA, f"Esb{i}", [128, 8, 2, 128], BF16) for i in range(2)]
            den = sb(esA, "den", [128, 8], F32)
            rden = sb(esA, "rden", [128, 8], F32)
            attn32 = [sb(esA, f"attn32{j}", [128, 512], F32) for j in range(GT)]
            ssa = [sb(esA, f"ssa{j}", [128, 1], F32) for j in range(GT)]
            lna = [sb(esA, f"lna{j}", [128, 1], F32) for j in range(GT)]
            rsta = [sb(esA, f"rsta{j}", [128, 1], F32) for j in range(GT)]
            attnbf = [sb(esA, f"attnbf{j}", [128, 512], BF16) for j in range(GT)]
            mixTs = [sb(esA, f"mixT{p}", [128, 8, GW], BF16) for p in range(2)]
            mixT_a = [[Res(f"mixTa{p}{j}") for j in range(GT)] for p in range(2)]
            mixT_c = [Res(f"mixTc{p}") for p in range(2)]
            ubufs = [sb(esA, f"ubuf{p}", [128, 4, HALO + GW], BF16) for p in range(2)]
            egb = [sb(esA, f"eg{i}", [128, GW], F32) for i in range(2)]
            v32 = sb(esA, "v32", [128, 4, GW], F32)
            v32_res = [Res(f"v32_{c}") for c in range(4)]
            vbf = sb(esA, "vbf", [128, 4, GW], BF16)
            vbf_res = [Res(f"vbf_{c}") for c in range(4)]
            sqbf = sb(esA, "sqbf", [128, 4, GW], BF16)
            sqbf_res = [Res(f"sqbf_{c}") for c in range(4)]
            mean_sb = sb(esA, "mean_sb", [128, GW], F32)
            m2_sb = sb(esA, "m2_sb", [128, GW], F32)
            rstd_sb = sb(esA, "rstd_sb", [128, GW], F32)
            r_sb = sb(esA, "r_sb", [128, GW], F32)
            ybuf = [sb(esA, f"ybuf{i}", [128, GW], F32) for i in range(2)]
            x1sb = [sb(esA, f"x1sb{j}", [128, D], F32) for j in range(GT)]
            ss1 = sb(esA, "ss1", [128, GT], F32)
            ln1 = sb(esA, "ln1", [128, GT], F32)
            rst1 = sb(esA, "rst1", [128, GT], F32)
            hfbf = [[sb(esA, f"hfbf{p}{j}", [128, D], BF16) for j in range(GT)] for p in range(2)]
            hfT = [sb(esA, f"hfT{j}", [128, 8, 128], BF16) for j in range(GT)]
            lgs = [sb(esA, f"lg{p}", [128, GT, 36], F32) for p in range(2)]
            gmax = sb(esA, "gmax", [128, GT], F32)
            ohg = sb(esA, "ohg", [128, GT, 4], F32)
            gsh = sb(esA, "gsh", [128, GT, 4], F32)
            gsum = sb(esA, "gsum", [128, GT], F32)
            gp = sb(esA, "gp", [128, GT], F32)
            tmp48 = sb(esA, "tmp48", [128, GT, 4, 8], F32)
            esel = sb(esA, "esel", [128, GT, 8], F32)
            m1 = sb(esA, "m1", [128, GT], F32)
            oh1 = sb(esA, "oh1", [128, GT, 8], F32)
            em = sb(esA, "em", [128, GT, 8], F32)
            m2 = sb(esA, "m2", [128, GT], F32)
            oh2 = sb(esA, "oh2", [128, GT, 8], F32)
            d21 = sb(esA, "d21", [128, GT], F32)
            e21 = sb(esA, "e21", [128, GT], F32)
            dn = sb(esA, "dn", [128, GT], F32)
            g1 = sb(esA, "g1", [128, GT], F32)
            g2 = sb(esA, "g2", [128, GT], F32)
            oh1fs = [sb(esA, f"oh1f{p}", [128, GT, 32], F32) for p in range(2)]
            oh2fs = [sb(esA, f"oh2f{p}", [128, GT, 32], F32) for p in range(2)]
            ohbs = [sb(esA, f"ohb{p}", [128, GT, 32], BF16) for p in range(2)]
            rk = sb(esA, "rk", [128, GT, 32], F32)
            t32 = sb(esA, "t32", [128, GT, 32], F32)

            zt = sb(esA, "zt", [128, D], BF16)
            V(lambda e: e.memset(zt.t[:], 0.0), [], [zt])
            zf_state = {"r": 0}

            def zero_fill(nchunks):
                for _ in range(nchunks):
                    r0 = zf_state["r"]
                    if r0 >= NROWS:
                        return
                    DG(lambda e, r0=r0: e.dma_start(out=xs_d[r0:r0 + 128, :], in_=zt.t[:]), [zt], [], multi=[xs_zero])
                    zf_state["r"] = r0 + 128
                    if r0 >= ((NTOK * 2 + BLK - 1) // BLK) * BLK:
                        DG(lambda e, r0=r0: e.dma_start(out=yb_d[r0:r0 + 128, :], in_=zt.t[:]), [zt], [], multi=[yb_zero])
            for p_ in range(2):
                V(lambda e, p_=p_: e.memset(ubufs[p_].t[:], 0.0), [], [ubufs[p_]])
            for i in range(5):
                V(lambda e, i=i: e.memset(vaug[i].t[:], 1.0), [], [vaug[i]])

            def load_x(G):
                p = G % 3
                for j in range(GT):
                    t = G * GT + j
                    DM(lambda e, p=p, j=j, t=t: e.dma_start(out=xsb[p][j].t[:], in_=x_d[t * 128:(t + 1) * 128, :]),
                       [], [xsb[p][j]])

            def stage_F(G):
                par = G % 2
                gs = G % (NT_SEQ // GT)
                load_x(G)
                xg = xsb[G % 3]
                ubuf = ubufs[par]
                ubuf_prev = ubufs[1 - par]
                for j in range(GT):
                    A(lambda e, j=j: e.activation(out=junk[:, :], in_=xg[j].t[:], func=AF.Square,
                                                  accum_out=ssx.t[:, j:j + 1]), [xg[j]], [ssx])
                A(lambda e: e.activation(out=lnx.t[:], in_=ssx.t[:], func=AF.Ln, scale=1.0 / D, bias=EPS),
                  [ssx], [lnx])
                A(lambda e: e.activation(out=rstx.t[:], in_=lnx.t[:], func=AF.Exp, scale=-0.5), [lnx], [rstx])
                for j in range(GT):
                    A(lambda e, j=j: e.activation(out=xsbf[j].t[:], in_=xg[j].t[:], func=AF.Copy, scale=rstx.t[:, j:j + 1]),
                      [xg[j], rstx], [xsbf[j]])
                yield
                hn = hnT[par]
                for half in range(2):
                    tp = next_tp()
                    for kk in range(4):
                        k = half * 4 + kk
                        for j in range(GT):
                            T(lambda e, tp=tp, kk=kk, j=j, k=k: e.transpose(
                                out=tp.t[:, kk * GW + j * 128: kk * GW + (j + 1) * 128],
                                in_=xsbf[j].t[:, k * 128:(k + 1) * 128], identity=ident),
                              [xsbf[j], cbf], [tp], inc=(kk == 3 and j == GT - 1))
                    fn = (lambda e, tp=tp, half=half: e.tensor_copy(
                        out=hn.t[:, half * 4:(half + 1) * 4, :].rearrange("p a b -> p (a b)"), in_=tp.t[:, :]))
                    if half == 0:
                        V(fn, [tp], [hn])
                    else:
                        A(lambda e, tp=tp, half=half: e.activation(
                            out=hn.t[:, half * 4:(half + 1) * 4, :].rearrange("p a b -> p (a b)"), in_=tp.t[:, :],
                            func=AF.Copy), [tp], [hn])
                yield
                Pq = [None] * GT
                slots = []
                for j in range(GT):
                    yield
                    t = G * GT + j
                    n = gs * GT + j
                    slot = t % 5
                    Pq[j] = next_mm()
                    Pkv = next_mm()
                    for k in range(8):
                        T(lambda e, j=j, k=k: e.matmul(Pq[j].t[:, :], lhsT=hn.t[:, k, j * 128:(j + 1) * 128],
                                                       rhs=w_in_bf.t[:, k, 0:512], start=(k == 0), stop=(k == 7)),
                          [hn, w_in_res[k]], [Pq[j]], inc=(k == 7))
                    for k in range(8):
                        T(lambda e, j=j, k=k, Pkv=Pkv: e.matmul(Pkv.t[:, 0:256],
                                                                 lhsT=hn.t[:, k, j * 128:(j + 1) * 128],
                                                                 rhs=w_in_bf.t[:, k, 512:768], start=(k == 0), stop=(k == 7)),
                          [hn, w_in_res[k]], [Pkv], inc=(k == 7))
                    slots.append(slot)
                    A(lambda e, j=j: e.activation(out=qsb[j].t[:], in_=Pq[j].t[:], func=AF.Copy), [Pq[j]], [qsb[j]])
                    q3 = Pq[j].t[:].rearrange("p (h d) -> p h d", h=8)
                    k3 = Pkv.t[:, 0:128].rearrange("p (h d) -> p h d", h=2)
                    Cb = lambda nh, t=t: ropeC.t[:, t, :].unsqueeze(1).to_broadcast([128, nh, 16])
                    Sa = lambda nh, t=t: ropeS.t[:, t, 0:8].unsqueeze(1).to_broadcast([128, nh, 8])
                    Sb = lambda nh, t=t: ropeS.t[:, t, 8:16].unsqueeze(1).to_broadcast([128, nh, 8])
                    V(lambda e, j=j, q3=q3, Cb=Cb: e.tensor_tensor(out=rt1[j].t[:, 0:8, :], in0=q3[:, :, 0:16], in1=Cb(8),
                                                                   op=ALU.mult), [Pq[j], ropeC], [rt1[j]])
                    V(lambda e, j=j, q3=q3, Sa=Sa: e.tensor_tensor(out=rt2[j].t[:, 0:8, 0:8], in0=q3[:, :, 8:16], in1=Sa(8),
                                                                   op=ALU.mult), [Pq[j], ropeS], [rt2[j]])
                    V(lambda e, j=j, q3=q3, Sb=Sb: e.tensor_tensor(out=rt2[j].t[:, 0:8, 8:16], in0=q3[:, :, 0:8], in1=Sb(8),
                                                                   op=ALU.mult), [Pq[j], ropeS], [rt2[j]])
                    V(lambda e, j=j, k3=k3, Cb=Cb: e.tensor_tensor(out=rt1[j].t[:, 8:10, :], in0=k3[:, :, 0:16], in1=Cb(2),
                                                                   op=ALU.mult), [Pkv, ropeC], [rt1[j]])
                    V(lambda e, j=j, k3=k3, Sa=Sa: e.tensor_tensor(out=rt2[j].t[:, 8:10, 0:8], in0=k3[:, :, 8:16], in1=Sa(2),
                                                                   op=ALU.mult), [Pkv, ropeS], [rt2[j]])
                    V(lambda e, j=j, k3=k3, Sb=Sb: e.tensor_tensor(out=rt2[j].t[:, 8:10, 8:16], in0=k3[:, :, 0:8], in1=Sb(2),
                                                                   op=ALU.mult), [Pkv, ropeS], [rt2[j]])
                    V(lambda e, j=j, Pkv=Pkv: e.tensor_copy(out=ksb[j].t[:], in_=Pkv.t[:, 0:128]), [Pkv], [ksb[j]])
                    V(lambda e, j=j, slot=slot: e.tensor_copy(
                        out=vaug[slot].t[:, :, 0:64],
                        in_=Pkv.t[:, 128:256].rearrange("p (h d) -> p h d", h=2)),
                      [Pkv], [vaug[slot]])
                    V(lambda e, j=j: e.tensor_tensor(
                        out=qsb[j].t[:].rearrange("p (h d) -> p h d", h=8)[:, :, 0:16],
                        in0=rt1[j].t[:, 0:8, :], in1=rt2[j].t[:, 0:8, :], op=ALU.add), [rt1[j], rt2[j]], [qsb[j]])
                    V(lambda e, j=j: e.tensor_tensor(
                        out=ksb[j].t[:].rearrange("p (h d) -> p h d", h=2)[:, :, 0:16],
                        in0=rt1[j].t[:, 8:10, :], in1=rt2[j].t[:, 8:10, :], op=ALU.add), [rt1[j], rt2[j]], [ksb[j]])
                    yield
                    yield
                    tp = next_tp()
                    for i in range(4):
                        T(lambda e, j=j, i=i, tp=tp: e.transpose(
                            out=tp.t[:, i * 128:(i + 1) * 128],
                            in_=qsb[j].t[:, i * 128:(i + 1) * 128],
                            identity=ident), [qsb[j], cbf], [tp], inc=False)
                    T(lambda e, j=j, tp=tp: e.transpose(out=tp.t[:, 512:640], in_=ksb[j].t[:], identity=ident),
                      [ksb[j], cbf], [tp])
                    V(lambda e, j=j, tp=tp: e.tensor_copy(out=qTs[par][j].t[:].rearrange("p a b -> p (a b)"), in_=tp.t[:, 0:512]),
                      [tp], [qTs[par][j]])
                    V(lambda e, slot=slot, tp=tp: e.tensor_copy(out=kTb[slot].t[:], in_=tp.t[:, 512:640]),
                      [tp], [kTb[slot]])
                yield
                if gs == 0:
                    V(lambda e: e.memset(ubuf.t[:, :, 0:HALO], 0.0), [], [ubuf])
                else:
                    V(lambda e: e.tensor_copy(out=ubuf.t[:, :, 0:HALO], in_=ubuf_prev.t[:, :, GW:GW + HALO]), [ubuf_prev], [ubuf])
                for c in range(4):
                    yield
                    Pag = next_mm()
                    for k in range(8):
                        T(lambda e, c=c, k=k, Pag=Pag: e.matmul(Pag.t[:, 0:GW], lhsT=w_in_bf.t[:, k, 768 + c * 128: 896 + c * 128],
                                                                 rhs=hn.t[:, k, :], start=(k == 0), stop=(k == 7)),
                          [hn, w_in_res[k]], [Pag], inc=False)
                    for k in range(8):
                        T(lambda e, c=c, k=k, Pag=Pag: e.matmul(Pag.t[:, GW:2 * GW], lhsT=w_in_bf.t[:, k, 1280 + c * 128: 1408 + c * 128],
                                                                 rhs=hn.t[:, k, :], start=(k == 0), stop=(k == 7)),
                          [hn, w_in_res[k]], [Pag], inc=(k == 7))
                    eg = egb[c % 2]
                    A(lambda e, Pag=Pag, eg=eg: e.activation(out=eg.t[:], in_=Pag.t[:, GW:2 * GW], func=AF.Exp, scale=-1.0),
                      [Pag], [eg])
                    A(lambda e, eg=eg: e.activation(out=eg.t[:], in_=eg.t[:], func=AF.Ln, bias=1.0, scale=1.0), [eg], [eg])
                    A(lambda e, eg=eg: e.activation(out=eg.t[:], in_=eg.t[:], func=AF.Exp, scale=-1.0), [eg], [eg])
                    V(lambda e, eg=eg, Pag=Pag, c=c: e.tensor_tensor(out=ubuf.t[:, c, HALO:HALO + GW], in0=Pag.t[:, 0:GW],
                                                                     in1=eg.t[:], op=ALU.mult), [Pag, eg, ubuf], [ubuf])

            def stage_Ma(G):
                par = G % 2
                gs = G % (NT_SEQ // GT)
                ubuf = ubufs[par]
                mixT = mixTs[par]

                def attn_tr(j):
                    tp = next_tp()
                    for i in range(4):
                        T(lambda e, j=j, i=i, tp=tp: e.transpose(out=tp.t[:, i * 128:(i + 1) * 128],
                                                                  in_=attnbf[j].t[:, i * 128:(i + 1) * 128], identity=ident),
                          [attnbf[j], cbf], [tp], inc=(i == 3))
                    V(lambda e, j=j, tp=tp: e.tensor_copy(
                        out=mixT.t[:, 0:4, j * 128:(j + 1) * 128],
                        in_=tp.t[:, 0:512].rearrange("p (a b) -> p a b", a=4)), [tp], [mixT_a[par][j]])
                for j in range(GT):
                    t = G * GT + j
                    n = gs * GT + j
                    slot = t % 5
                    pslot = (t - 1) % 5
                    Eb = Esb[t % 2]
                    for hp in range(4):
                        yield
                        if hp == 2 and j > 0:
                            attn_tr(j - 1)
                        STb = next_mm()
                        for hh in range(2):
                            h = 2 * hp + hh
                            kv = h // 4
                            i = h % 4
                            rows = slice(kv * 64, kv * 64 + 64)
                            base = hh * 256
                            if n > 0:
                                T(lambda e, STb=STb, rows=rows, i=i, j=j, base=base, pslot=pslot: e.matmul(
                                    STb.t[:, base: base + 128], lhsT=kTb[pslot].t[rows, :], rhs=qTs[par][j].t[rows, i, :],
                                    start=True, stop=False), [kTb[pslot], qTs[par][j]], [STb], inc=False)
                                T(lambda e, STb=STb, base=base: e.matmul(
                                    STb.t[:, base: base + 128], lhsT=ident, rhs=maskprev, start=False, stop=True),
                                  [cbf], [STb], inc=False)
                            T(lambda e, STb=STb, rows=rows, i=i, j=j, base=base, slot=slot: e.matmul(
                                STb.t[:, base + 128: base + 256], lhsT=kTb[slot].t[rows, :], rhs=qTs[par][j].t[rows, i, :],
                                start=True, stop=False), [kTb[slot], qTs[par][j]], [STb], inc=False)
                            T(lambda e, STb=STb, base=base: e.matmul(
                                STb.t[:, base + 128: base + 256], lhsT=ident, rhs=maskcur, start=False, stop=True),
                              [cbf], [STb], inc=(hh == 1))
                        if n > 0:
                            A(lambda e, STb=STb, Eb=Eb, hp=hp: e.activation(
                                out=Eb.t[:, 2 * hp:2 * hp + 2, :, :].rearrange("p a b c -> p (a b c)"), in_=STb.t[:, :],
                                func=AF.Exp, scale=0.125), [STb], [Eb])
                        else:
                            A(lambda e, STb=STb, Eb=Eb, hp=hp: e.activation(
                                out=Eb.t[:, 2 * hp:2 * hp + 2, 1, :],
                                in_=STb.t[:, :].rearrange("p (a b c) -> p a b c", a=2, b=2)[:, :, 1, :],
                                func=AF.Exp, scale=0.125), [STb], [Eb])
                    yield
                    yield
                    PO = [next_mm(), next_mm()]
                    for h in range(8):
                        kv = h // 4
                        ob = PO[h // 4]
                        oc = (h % 4) * 128
                        if n > 0:
                            T(lambda e, ob=ob, oc=oc, h=h, kv=kv, Eb=Eb, pslot=pslot: e.matmul(
                                ob.t[:, oc: oc + 65], lhsT=Eb.t[:, h, 0, :], rhs=vaug[pslot].t[:, kv, :],
                                start=True, stop=False), [Eb, vaug[pslot]], [ob], inc=False)
                        T(lambda e, ob=ob, oc=oc, h=h, kv=kv, Eb=Eb, slot=slot, n=n: e.matmul(
                            ob.t[:, oc: oc + 65], lhsT=Eb.t[:, h, 1, :], rhs=vaug[slot].t[:, kv, :],
                            start=(n == 0), stop=True), [Eb, vaug[slot]], [ob], inc=(h % 4 == 3))
                    for hb in range(2):
                        o3 = PO[hb].t[:].rearrange("p (h d) -> p h d", h=4)
                        V(lambda e, o3=o3, hb=hb: e.tensor_tensor(out=den.t[:, hb * 4:(hb + 1) * 4], in0=o3[:, :, 64],
                                                                   in1=esink.t[:, hb * 4:(hb + 1) * 4], op=ALU.add),
                          [PO[hb], esink], [den])
                    V(lambda e: e.reciprocal(out=rden.t[:], in_=den.t[:]), [den], [rden])
                    for hb in range(2):
                        o3 = PO[hb].t[:].rearrange("p (h d) -> p h d", h=4)
                        V(lambda e, o3=o3, hb=hb, j=j: e.tensor_tensor(
                            out=attn32[j].t[:, hb * 256:(hb + 1) * 256].rearrange("p (h d) -> p h d", h=4),
                            in0=o3[:, :, 0:64],
                            in1=rden.t[:, hb * 4:(hb + 1) * 4].unsqueeze(2).to_broadcast([128, 4, 64]),
                            op=ALU.mult), [PO[hb], rden], [attn32[j]])
                    A(lambda e, j=j: e.activation(out=junk[:, 0:512], in_=attn32[j].t[:], func=AF.Square,
                                                  accum_out=ssa[j].t[:]), [attn32[j]], [ssa[j]])
                    A(lambda e, j=j: e.activation(out=lna[j].t[:], in_=ssa[j].t[:], func=AF.Ln, scale=1.0 / 512, bias=EPS),
                      [ssa[j]], [lna[j]])
                    A(lambda e, j=j: e.activation(out=rsta[j].t[:], in_=lna[j].t[:], func=AF.Exp, scale=-0.5), [lna[j]], [rsta[j]])
                    A(lambda e, j=j: e.activation(out=attnbf[j].t[:], in_=attn32[j].t[:], func=AF.Copy, scale=rsta[j].t[:, 0:1]),
                      [attn32[j], rsta[j]], [attnbf[j]])
                yield
                yield
                attn_tr(GT - 1)

            def stage_Mc(G):
                par = G % 2
                gs = G % (NT_SEQ // GT)
                ubuf = ubufs[par]
                mixT = mixTs[par]
                for c in range(4):
                    yield
                    Pv = next_mm()
                    for jt in range(KTAPS):
                        T(lambda e, c=c, jt=jt, Pv=Pv: e.matmul(Pv.t[:, 0:GW], lhsT=diag.t[:, c, jt, :],
                                                                 rhs=ubuf.t[:, c, jt: jt + GW], start=(jt == 0),
                                                                 stop=(jt == KTAPS - 1)),
                          [diag_res[c], ubuf], [Pv], inc=(jt == KTAPS - 1))
                    A(lambda e, c=c, Pv=Pv: e.activation(out=v32.t[:, c, :], in_=Pv.t[:, 0:GW], func=AF.Identity,
                                                         bias=vfm.t[:, 24 + c:25 + c], scale=1.0), [Pv, vfm], [v32_res[c]])
                    A(lambda e, c=c, Pv=Pv: e.activation(out=vbf.t[:, c, :], in_=Pv.t[:, 0:GW], func=AF.Identity,
                                                         bias=vfm.t[:, 24 + c:25 + c], scale=1.0), [Pv, vfm], [vbf_res[c]])
                    A(lambda e, c=c, Pv=Pv: e.activation(out=sqbf.t[:, c, :], in_=Pv.t[:, 0:GW], func=AF.Square,
                                                         bias=vfm.t[:, 24 + c:25 + c], scale=1.0), [Pv, vfm], [sqbf_res[c]])
                yield
                yield
                Pst = next_mm()
                for c in range(4):
                    T(lambda e, c=c: e.matmul(Pst.t[:, 0:GW], lhsT=onesm, rhs=vbf.t[:, c, :], start=(c == 0), stop=(c == 3)),
                      [vbf_res[c], cbf], [Pst], inc=False)
                for c in range(4):
                    T(lambda e, c=c: e.matmul(Pst.t[:, GW:2 * GW], lhsT=onesm, rhs=sqbf.t[:, c, :], start=(c == 0), stop=(c == 3)),
                      [sqbf_res[c], cbf], [Pst], inc=(c == 3))
                V(lambda e: e.tensor_copy(out=mean_sb.t[:], in_=Pst.t[:, 0:GW]), [Pst], [mean_sb])
                A(lambda e: e.activation(out=m2_sb.t[:], in_=Pst.t[:, 0:GW], func=AF.Square), [Pst], [m2_sb])
                V(lambda e: e.tensor_tensor(out=rstd_sb.t[:], in0=Pst.t[:, GW:2 * GW], in1=m2_sb.t[:], op=ALU.subtract),
                  [Pst, m2_sb], [rstd_sb])
                A(lambda e: e.activation(out=rstd_sb.t[:], in_=rstd_sb.t[:], func=AF.Ln, bias=EPS, scale=1.0), [rstd_sb], [rstd_sb])
                A(lambda e: e.activation(out=rstd_sb.t[:], in_=rstd_sb.t[:], func=AF.Exp, scale=-0.5), [rstd_sb], [rstd_sb])
                yield
                for c in range(4):
                    yield
                    yb_ = ybuf[c % 2]
                    eg = egb[c % 2]
                    V(lambda e, c=c: e.tensor_tensor(out=v32.t[:, c, :], in0=v32.t[:, c, :], in1=mean_sb.t[:], op=ALU.subtract),
                      [v32_res[c], mean_sb], [v32_res[c]])
                    V(lambda e, c=c: e.tensor_tensor(out=v32.t[:, c, :], in0=v32.t[:, c, :], in1=rstd_sb.t[:], op=ALU.mult),
                      [v32_res[c], rstd_sb], [v32_res[c]])
                    V(lambda e, c=c, yb_=yb_: e.tensor_scalar(out=yb_.t[:], in0=v32.t[:, c, :], scalar1=vfm.t[:, 28 + c:29 + c],
                                                              scalar2=vfm.t[:, 32 + c:33 + c], op0=ALU.mult, op1=ALU.add),
                      [v32_res[c], vfm], [yb_])
                    A(lambda e, c=c, eg=eg: e.activation(out=eg.t[:], in_=v32.t[:, c, :], func=AF.Exp,
                                                         scale=nln.t[:, c:c + 1], bias=nln.t[:, 4 + c:5 + c]),
                      [v32_res[c], nln], [eg])
                    A(lambda e, eg=eg: e.activation(out=eg.t[:], in_=eg.t[:], func=AF.Ln, bias=1.0, scale=1.0), [eg], [eg])
                    A(lambda e, eg=eg: e.activation(out=eg.t[:], in_=eg.t[:], func=AF.Exp, scale=-1.0), [eg], [eg])
                    V(lambda e, c=c, eg=eg, yb_=yb_: e.tensor_tensor(out=v32.t[:, c, :], in0=yb_.t[:], in1=eg.t[:], op=ALU.mult),
                      [yb_, eg, v32_res[c]], [v32_res[c]])
                    A(lambda e, c=c: e.activation(out=sqbf.t[:, c, :], in_=v32.t[:, c, :], func=AF.Square),
                      [v32_res[c]], [sqbf_res[c]])
                yield
                yield
                yield
                Pms = next_mm()
                for c in range(4):
                    T(lambda e, c=c: e.matmul(Pms.t[:, 0:GW], lhsT=onesm, rhs=sqbf.t[:, c, :], start=(c == 0), stop=(c == 3)),
                      [sqbf_res[c], cbf], [Pms], inc=(c == 3))
                A(lambda e: e.activation(out=r_sb.t[:], in_=Pms.t[:, 0:GW], func=AF.Ln, bias=EPS, scale=1.0), [Pms], [r_sb])
                A(lambda e: e.activation(out=r_sb.t[:], in_=r_sb.t[:], func=AF.Exp, scale=-0.5), [r_sb], [r_sb])
                yield
                for c in range(4):
                    S.op("dve", lambda e, c=c: e.tensor_tensor(out=mixT.t[:, 4 + c, :], in0=v32.t[:, c, :], in1=r_sb.t[:], op=ALU.mult),
                         [v32_res[c], r_sb], [], multi=[mixT_c[par]])

            def stage_E(G):
                par = G % 2
                xg = xsb[G % 3]
                mixT = mixTs[par]
                lg = lgs[par]
                zero_fill((NROWS // 128 + NG - 1) // NG)
                for j in range(GT):
                    yield
                    t = G * GT + j
                    Po = [next_mm(), next_mm()]
                    for hf_ in range(2):
                        for k in range(8):
                            rd = [w_out_res[k], mixT_a[par][j] if k < 4 else mixT_c[par]]
                            T(lambda e, hf_=hf_, k=k, j=j: e.matmul(Po[hf_].t[:, :], lhsT=mixT.t[:, k, j * 128:(j + 1) * 128],
                                                                     rhs=w_out_bf.t[:, k, hf_ * 512:(hf_ + 1) * 512],
                                                                     start=(k == 0), stop=(k == 7)),
                              rd, [Po[hf_]], inc=(k == 7))
                    for hf_ in range(2):
                        V(lambda e, hf_=hf_, j=j: e.tensor_tensor(out=x1sb[j].t[:, hf_ * 512:(hf_ + 1) * 512],
                                                                   in0=Po[hf_].t[:, :], in1=xg[j].t[:, hf_ * 512:(hf_ + 1) * 512],
                                                                   op=ALU.add), [Po[hf_], xg[j]], [x1sb[j]])
                    DG(lambda e, j=j, t=t: e.dma_start(out=x1_d[t * 128:(t + 1) * 128, :], in_=x1sb[j].t[:]),
                       [x1sb[j]], [x1_res[t]])
                    A(lambda e, j=j: e.activation(out=junk[:, :], in_=x1sb[j].t[:], func=AF.Square,
                                                  accum_out=ss1.t[:, j:j + 1]), [x1sb[j]], [ss1])
                yield
                A(lambda e: e.activation(out=ln1.t[:], in_=ss1.t[:], func=AF.Ln, scale=1.0 / D, bias=EPS), [ss1], [ln1])
                A(lambda e: e.activation(out=rst1.t[:], in_=ln1.t[:], func=AF.Exp, scale=-0.5), [ln1], [rst1])
                for j in range(GT):
                    hb_ = hfbf[par][j]
                    V(lambda e, j=j, hb_=hb_: e.scalar_tensor_tensor(out=hb_.t[:], in0=x1sb[j].t[:], scalar=rst1.t[:, j:j + 1],
                                                                      in1=ffw_bc, op0=ALU.mult, op1=ALU.mult),
                      [x1sb[j], rst1, vbc], [hb_])
                for j in range(GT):
                    yield
                    yield
                    hb_ = hfbf[par][j]
                    tp = next_tp()
                    for k in range(8):
                        T(lambda e, k=k, tp=tp, hb_=hb_: e.transpose(out=tp.t[:, k * 128:(k + 1) * 128],
                                                                      in_=hb_.t[:, k * 128:(k + 1) * 128], identity=ident),
                          [hb_, cbf], [tp], inc=(k == 7))
                    A(lambda e, j=j, tp=tp: e.activation(out=hfT[j].t[:].rearrange("p a b -> p (a b)"), in_=tp.t[:, :],
                                                         func=AF.Copy), [tp], [hfT[j]])
                for j in range(GT):
                    yield
                    Pr = next_mm()
                    for k in range(8):
                        T(lambda e, j=j, k=k, Pr=Pr: e.matmul(Pr.t[:, 0:36], lhsT=hfT[j].t[:, k, :], rhs=wr_bf.t[:, k, :],
                                                               start=(k == 0), stop=(k == 7)), [hfT[j], wr_bf], [Pr], inc=(k == 7))
                    V(lambda e, j=j, Pr=Pr: e.tensor_tensor(out=lg.t[:, j, :], in0=Pr.t[:, 0:36], in1=rbias, op=ALU.add),
                      [Pr, vbc], [lg])
                yield
                t0 = G * GT
                for j in range(GT):
                    t = t0 + j
                    DG(lambda e, j=j, t=t: e.dma_start(out=hf_d[t * 128:(t + 1) * 128, :], in_=hfbf[par][j].t[:]),
                       [hfbf[par][j]], [hf_res[t]])

            def stage_R1(G):
                par = G % 2
                lg = lgs[par]
                oh1f, oh2f, ohb = oh1fs[par], oh2fs[par], ohbs[par]
                R3 = lambda ap, n_: ap.unsqueeze(2).to_broadcast([128, GT, n_])
                V(lambda e: e.tensor_reduce(out=gmax.t[:], in_=lg.t[:, :, 0:4], axis=AX.X, op=ALU.max), [lg], [gmax])
                V(lambda e: e.tensor_tensor(out=ohg.t[:], in0=lg.t[:, :, 0:4], in1=R3(gmax.t[:], 4), op=ALU.is_equal),
                  [lg, gmax], [ohg])
                V(lambda e: e.tensor_tensor(out=gsh.t[:], in0=lg.t[:, :, 0:4], in1=R3(gmax.t[:], 4), op=ALU.subtract),
                  [lg, gmax], [gsh])
                A(lambda e: e.activation(out=gsh.t[:], in_=gsh.t[:], func=AF.Exp), [gsh], [gsh])
                V(lambda e: e.tensor_reduce(out=gsum.t[:], in_=gsh.t[:], axis=AX.X, op=ALU.add), [gsh], [gsum])
                V(lambda e: e.reciprocal(out=gp.t[:], in_=gsum.t[:]), [gsum], [gp])
                yield
                V(lambda e: e.tensor_tensor(out=tmp48.t[:], in0=lg.t[:, :, 4:36].rearrange("p j (g c) -> p j g c", g=4),
                                            in1=ohg.t[:].unsqueeze(3).to_broadcast([128, GT, 4, 8]), op=ALU.mult),
                  [lg, ohg], [tmp48])
                V(lambda e: e.tensor_reduce(out=esel.t[:], in_=tmp48.t[:].rearrange("p j g c -> p j c g"), axis=AX.X, op=ALU.add),
                  [tmp48], [esel])
                V(lambda e: e.tensor_reduce(out=m1.t[:], in_=esel.t[:], axis=AX.X, op=ALU.max), [esel], [m1])
                V(lambda e: e.tensor_tensor(out=oh1.t[:], in0=esel.t[:], in1=R3(m1.t[:], 8), op=ALU.is_equal), [esel, m1], [oh1])
                V(lambda e: e.scalar_tensor_tensor(out=em.t[:], in0=oh1.t[:], scalar=-1e30, in1=esel.t[:], op0=ALU.mult, op1=ALU.add),
                  [oh1, esel], [em])
                V(lambda e: e.tensor_reduce(out=m2.t[:], in_=em.t[:], axis=AX.X, op=ALU.max), [em], [m2])
                V(lambda e: e.tensor_tensor(out=oh2.t[:], in0=em.t[:], in1=R3(m2.t[:], 8), op=ALU.is_equal), [em, m2], [oh2])
                yield
                V(lambda e: e.tensor_tensor(out=d21.t[:], in0=m2.t[:], in1=m1.t[:], op=ALU.subtract), [m1, m2], [d21])
                A(lambda e: e.activation(out=e21.t[:], in_=d21.t[:], func=AF.Exp), [d21], [e21])
                V(lambda e: e.tensor_scalar(out=dn.t[:], in0=e21.t[:], scalar1=1.0, scalar2=None, op0=ALU.add), [e21], [dn])
                V(lambda e: e.reciprocal(out=dn.t[:], in_=dn.t[:]), [dn], [dn])
                V(lambda e: e.tensor_tensor(out=g1.t[:], in0=gp.t[:], in1=dn.t[:], op=ALU.mult), [gp, dn], [g1])
                V(lambda e: e.tensor_tensor(out=g2.t[:], in0=g1.t[:], in1=e21.t[:], op=ALU.mult), [g1, e21], [g2])
                yield
                for (ohx, ohxf) in ((oh1, oh1f), (oh2, oh2f)):
                    V(lambda e, ohx=ohx, ohxf=ohxf: e.tensor_tensor(
                        out=ohxf.t[:].rearrange("p j (g c) -> p j g c", g=4),
                        in0=ohg.t[:].unsqueeze(3).to_broadcast([128, GT, 4, 8]),
                        in1=ohx.t[:].unsqueeze(2).to_broadcast([128, GT, 4, 8]), op=ALU.mult), [ohg, ohx], [ohxf])
                V(lambda e: e.tensor_tensor(out=ohb.t[:], in0=oh1f.t[:], in1=oh2f.t[:], op=ALU.add), [oh1f, oh2f], [ohb])
                t0 = G * GT
                V(lambda e: e.tensor_copy(out=gates.t[:, t0:t0 + GT, 0], in_=g1.t[:]), [g1], [gates])
                V(lambda e: e.tensor_copy(out=gates.t[:, t0:t0 + GT, 1], in_=g2.t[:]), [g2], [gates])

            def stage_R2(G):
                par = G % 2
                oh1f, oh2f, ohb = oh1fs[par], oh2fs[par], ohbs[par]
                yield
                Prk = next_mm()
                for j in range(GT):
                    T(lambda e, j=j: e.matmul(Prk.t[:, j * 32:(j + 1) * 32], lhsT=Utri, rhs=ohb.t[:, j, :], start=True,
                                              stop=(j == 0)), [ohb, cbf], [Prk], inc=False)
                    for jj in range(j):
                        T(lambda e, j=j, jj=jj: e.matmul(Prk.t[:, j * 32:(j + 1) * 32], lhsT=ones_b, rhs=ohb.t[:, jj, :],
                                                         start=False, stop=(jj == j - 1)), [ohb, cbf], [Prk], inc=False)
                for j in range(GT):
                    T(lambda e, j=j: e.matmul(Prk.t[:, 256:288], lhsT=ones_b, rhs=ohb.t[:, j, :], start=(j == 0),
                                              stop=(j == GT - 1)), [ohb, cbf], [Prk], inc=(j == GT - 1))
                V(lambda e: e.tensor_tensor(out=rk.t[:], in0=Prk.t[:, 0:GT * 32].rearrange("p (j c) -> p j c", j=GT),
                                            in1=cnt.t[:].unsqueeze(1).to_broadcast([128, GT, 32]), op=ALU.add), [Prk, cnt], [rk])
                V(lambda e: e.tensor_tensor(out=cnt.t[:], in0=cnt.t[:], in1=Prk.t[:, 256:288], op=ALU.add), [Prk, cnt], [cnt])
                t0 = G * GT
                for ci_, ohxf in enumerate((oh1f, oh2f)):
                    V(lambda e, ohxf=ohxf: e.tensor_tensor(out=t32.t[:], in0=ohxf.t[:], in1=rk.t[:], op=ALU.mult), [ohxf, rk], [t32])
                    V(lambda e, ci_=ci_: e.tensor_reduce(out=rkf.t[:, t0:t0 + GT, ci_], in_=t32.t[:], axis=AX.X, op=ALU.add), [t32], [rkf])
                    V(lambda e, ohxf=ohxf: e.tensor_tensor(out=t32.t[:], in0=ohxf.t[:],
                                                           in1=iota_bc.unsqueeze(1).to_broadcast([128, GT, 32]), op=ALU.mult),
                      [ohxf, vbc], [t32])
                    V(lambda e, ci_=ci_: e.tensor_reduce(out=exf.t[:, t0:t0 + GT, ci_], in_=t32.t[:], axis=AX.X, op=ALU.add), [t32], [exf])
            NGr = NGrun if stop != 'p0' else 0
            if NGr < NG:
                zero_fill(NROWS // 128)
            for step in range(NGr + 4):
                gens = []
                if 0 <= step - 1 < NGr:
                    gens.append(stage_Ma(step - 1))
                    gens.append(stage_Mc(step - 1))
                if step < NGr:
                    gens.append(stage_F(step))
                if 0 <= step - 2 < NGr:
                    gens.append(stage_E(step - 2))
                if 0 <= step - 3 < NGr:
                    gens.append(stage_R1(step - 3))
                if 0 <= step - 4 < NGr:
                    gens.append(stage_R2(step - 4))
                while gens:
                    for g_ in list(gens):
                        try:
                            next(g_)
                        except StopIteration:
                            gens.remove(g_)
            S.barrier()

        with ExitStack() as esB:
          if stop is None or stop in ('B', 'C'):
              nbf = sb(esB, "nbf", [128, 32], F32)
              nbi = sb(esB, "nbi", [128, 32], I32)
              padded = sb(esB, "padded", [128, 32], F32)
              cumi = sb(esB, "cumi", [128, 32], F32)
              pstart = sb(esB, "pstart", [128, 32], F32)
              cmpb = sb(esB, "cmpb", [128, 64, 32], F32)
              bef = sb(esB, "bef", [128, 64], F32)
              CH = 16
              ohc = sb(esB, "ohc", [128, CH * 2, 32], F32)
              dsf = sb(esB, "dsf", [128, NT * 2], F32)
              hrow = [sb(esB, f"hrow{i}", [128, D], BF16) for i in range(8)]
              V(lambda e: e.tensor_scalar(out=nbf.t[:], in0=cnt.t[:], scalar1=1.0 / BLK,
                                          scalar2=float((BLK - 1) / BLK - 0.5 + 0.5 / BLK), op0=ALU.mult, op1=ALU.add),
                [cnt], [nbf])
              V(lambda e: e.tensor_copy(out=nbi.t[:], in_=nbf.t[:]), [nbf], [nbi])
              V(lambda e: e.tensor_copy(out=nbf.t[:], in_=nbi.t[:]), [nbi], [nbf])
              V(lambda e: e.tensor_scalar(out=padded.t[:], in0=nbf.t[:], scalar1=float(BLK), scalar2=None, op0=ALU.mult),
                [nbf], [padded])
              V(lambda e: e.tensor_copy(out=cumi.t[:], in_=padded.t[:]), [padded], [cumi])
              for e_ in range(1, NE):
                  V(lambda e, e_=e_: e.tensor_tensor(out=cumi.t[:, e_:e_ + 1], in0=cumi.t[:, e_ - 1:e_], in1=padded.t[:, e_:e_ + 1],
                                                     op=ALU.add), [cumi, padded], [cumi])
              V(lambda e: e.tensor_tensor(out=pstart.t[:], in0=cumi.t[:], in1=padded.t[:], op=ALU.subtract), [cumi, padded], [pstart])
              V(lambda e: e.tensor_tensor(out=cmpb.t[:], in0=cumi.t[:].unsqueeze(1).to_broadcast([128, 64, 32]),
                                          in1=bstart_bc.unsqueeze(2).to_broadcast([128, 64, 32]), op=ALU.is_le),
                [cumi, vbc], [cmpb])
              V(lambda e: e.tensor_reduce(out=bef.t[:], in_=cmpb.t[:], axis=AX.X, op=ALU.add), [cmpb], [bef])
              V(lambda e: e.tensor_scalar(out=bef.t[:], in0=bef.t[:], scalar1=float(NE - 1), scalar2=None, op0=ALU.min), [bef], [bef])
              V(lambda e: e.tensor_scalar(out=bef.t[:], in0=bef.t[:], scalar1=128.0, scalar2=vfm.t[:, 160:161],
                                          op0=ALU.mult, op1=ALU.add), [bef, vfm], [bef])
              unusedf = sb(esB, "unusedf", [128, 64], F32)
              nuf = sb(esB, "nuf", [128, 1], F32)
              V(lambda e: e.tensor_scalar(out=unusedf.t[:], in0=bstart_bc, scalar1=cumi.t[:, NE - 1:NE], scalar2=None,
                                          op0=ALU.is_ge), [cumi, vbc], [unusedf])
              V(lambda e: e.scalar_tensor_tensor(out=bef.t[:], in0=unusedf.t[:], scalar=16384.0, in1=bef.t[:], op0=ALU.mult,
                                                 op1=ALU.add), [unusedf, bef], [bef])
              V(lambda e: e.tensor_copy(out=widx.t[:], in_=bef.t[:]), [bef], [widx])
              V(lambda e: e.tensor_scalar(out=nuf.t[:], in0=cumi.t[:, NE - 1:NE], scalar1=1.0 / BLK, scalar2=None, op0=ALU.mult),
                [cumi], [nuf])
              V(lambda e: e.tensor_copy(out=nused_i.t[:], in_=nuf.t[:]), [nuf], [nused_i])
              exv = exf.t[:].rearrange("p t c -> p (t c)")
              rkv = rkf.t[:].rearrange("p t c -> p (t c)")
              for c0 in range(0, NT * 2, CH * 2):
                  n_ = min(CH * 2, NT * 2 - c0)
                  V(lambda e, c0=c0, n_=n_: e.tensor_tensor(out=ohc.t[:, 0:n_, :],
                                                            in0=iota_bc.unsqueeze(1).to_broadcast([128, n_, 32]),
                                                            in1=exv[:, c0:c0 + n_].unsqueeze(2).to_broadcast([128, n_, 32]),
                                                            op=ALU.is_equal), [exf, vbc], [ohc])
                  V(lambda e, n_=n_: e.tensor_tensor(out=ohc.t[:, 0:n_, :], in0=ohc.t[:, 0:n_, :],
                                                     in1=pstart.t[:].unsqueeze(1).to_broadcast([128, n_, 32]), op=ALU.mult),
                    [ohc, pstart], [ohc])
                  V(lambda e, c0=c0, n_=n_: e.tensor_reduce(out=dsf.t[:, c0:c0 + n_], in_=ohc.t[:, 0:n_, :], axis=AX.X, op=ALU.add),
                    [ohc], [dsf])
              V(lambda e: e.tensor_tensor(out=dsf.t[:], in0=dsf.t[:], in1=rkv, op=ALU.add), [dsf, rkf], [dsf])
              V(lambda e: e.tensor_copy(out=dest.t[:].rearrange("p t c -> p (t c)"), in_=dsf.t[:]), [dsf], [dest])
              if debug:
                  dbg = sb(esB, "dbgsb", [128, NT, 4], F32)
                  V(lambda e: e.tensor_copy(out=dbg.t[:, :, 0:2], in_=exf.t[:]), [exf], [dbg])
                  V(lambda e: e.tensor_copy(out=dbg.t[:, :, 2:4], in_=gates.t[:]), [gates, dbg], [dbg])
                  DM(lambda e: e.dma_start(out=dbg_d, in_=dbg.t[:]), [dbg], [])
              for t in range(NT):
                  hr = hrow[t % 8]
                  DM(lambda e, t=t, hr=hr: e.dma_start(out=hr.t[:], in_=hf_d[t * 128:(t + 1) * 128, :]), [hf_res[t]], [hr])
                  for ch in range(2):
                      DG(lambda e, t=t, ch=ch, hr=hr: e.indirect_dma_start(
                          out=xs_d[:, :], out_offset=bass.IndirectOffsetOnAxis(ap=dest.t[:, t, ch:ch + 1], axis=0),
                          in_=hr.t[:, :], in_offset=None, bounds_check=bc_reg, oob_is_err=False),
                         [dest, hr, xs_zero], [], multi=[xs_res])
              S.barrier()

        with ExitStack() as esC:
          if stop is None or stop == 'C':
              wg_bf = [sb(esC, f"wg{p}", [128, 8, DE], BF16) for p in range(2)]
              wu_bf = [sb(esC, f"wu{p}", [128, 8, DE], BF16) for p in range(2)]
              wd_bf = [sb(esC, f"wd{p}", [128, 4, D], BF16) for p in range(2)]
              wstg = [[sb(esC, f"wstg{q}{i}", [128, 4096], F32) for i in range(3)] for q in range(2)]
              xrow = [sb(esC, f"xrow{p}", [128, RT, D], BF16) for p in range(2)]
              xT = sb(esC, "xT", [128, 8, BLK], BF16)
              xT_res = [Res(f"xT{k}") for k in range(8)]
              hT = sb(esC, "hT", [128, 4, BLK], BF16)
              hT_res = [Res(f"hT{m}") for m in range(4)]
              sil = [sb(esC, f"sil{i}", [128, 512], F32) for i in range(2)]
              yo = [sb(esC, f"yo{i}", [128, D], BF16) for i in range(4)]
              tpc = [ps(esC, f"tpc{i}", [128, 1024], BF16) for i in range(2)]
              mc = [ps(esC, f"mc{i}", [128, 512], F32) for i in range(6)]
              stc = {"mm": 0, "tp": 0, "stg": 0, "cast": 0, "yo": 0}

              def nmm():
                  b = mc[stc["mm"] % 6]; stc["mm"] += 1; return b

              def ntp():
                  b = tpc[stc["tp"] % 2]; stc["tp"] += 1; return b

              def cast(out_ap, in_ap, rd, wr_multi):
                  eng = ("act", "act", "dve")[stc["cast"] % 3]
                  stc["cast"] += 1
                  if eng == "act":
                      A(lambda e: e.activation(out=out_ap, in_=in_ap, func=AF.Copy), rd, [], multi=wr_multi)
                  else:
                      S.op(eng, lambda e: e.tensor_copy(out=out_ap, in_=in_ap), rd, [], multi=wr_multi)

              wsrc = [wg_d.rearrange("e (q k) c -> (e q) (k c)", k=8),
                      wu_d.rearrange("e (q k) c -> (e q) (k c)", k=8),
                      wd_d.rearrange("e (q m) c -> (e q) (m c)", m=4)]

              def load_block(b):
                  p = b % 2
                  for i in range(3):
                      DG(lambda e, i=i: e.indirect_dma_start(
                          out=wstg[p][i].t[:, :], out_offset=None, in_=wsrc[i][:, :],
                          in_offset=bass.IndirectOffsetOnAxis(ap=widx.t[:, b:b + 1], axis=0),
                          bounds_check=bcw_reg, oob_is_err=False), [widx], [wstg[p][i]])

              def load_x_block(b):
                  p = b % 2
                  DM(lambda e: e.dma_start(out=xrow[p].t[:],
                                           in_=xs_d[b * BLK:(b + 1) * BLK, :].rearrange("(r p) c -> p r c", p=128)),
                     [xs_res], [xrow[p]])

              def cast_block(b):
                  p = b % 2
                  dsts = [wg_bf[p], wu_bf[p], wd_bf[p]]
                  for i in range(3):
                      dflat = dsts[i].t[:].rearrange("p a c -> p (a c)")
                      for pc in range(4):
                          cast(dflat[:, pc * 1024:(pc + 1) * 1024], wstg[p][i].t[:, pc * 1024:(pc + 1) * 1024], [wstg[p][i]], [dsts[i]])

              BMIN = (NTOK * 2 + BLK - 1) // BLK
              SKIP_ENGS = ("pe", "act", "dve", "sp")
              nu_reg = {}
              for q_ in SKIP_ENGS:
                  nu_reg[q_] = S.eng[q_].alloc_register("nused_" + q_)
                  S.wait_all(q_, [nused_i])
                  S.eng[q_].reg_load(nu_reg[q_], nused_i.t[0:1, 0:1])
              load_block(0)
              if NBLK > 1:
                  load_block(1)
              load_x_block(0)
              cast_block(0)
              for b in range(NBLK):
                  p = b % 2
                  if b + 2 < NBLK:
                      load_block(b + 2)
                  if b + 1 < NBLK:
                      load_x_block(b + 1)
                  ctx = None
                  if b >= BMIN and os.environ.get("K_NOSKIP") != "1":
                      snap = {k_: v_ for k_, v_ in S.waited.items() if k_[0] in SKIP_ENGS}
                      n0 = {q_: S.ccnt[q_] for q_ in SKIP_ENGS if q_ != "sp"}
                      ctx = True
                      S.defer = {q_: [] for q_ in SKIP_ENGS}
                      S.defer_dma = []
                  for k in range(8):
                      tp = ntp()
                      for r in range(RT):
                          T(lambda e, tp=tp, r=r, k=k: e.transpose(out=tp.t[:, r * 128:(r + 1) * 128],
                                                                    in_=xrow[p].t[:, r, :].rearrange("t (q k) -> t k q", k=8)[:, k, :],
                                                                    identity=ident), [xrow[p], cbf], [tp], inc=(r == RT - 1))
                      if k % 2 == 0:
                          V(lambda e, tp=tp, k=k: e.tensor_copy(out=xT.t[:, k, :], in_=tp.t[:, 0:BLK]), [tp], [], multi=[xT_res[k]])
                      else:
                          A(lambda e, tp=tp, k=k: e.activation(out=xT.t[:, k, :], in_=tp.t[:, 0:BLK], func=AF.Copy), [tp], [],
                            multi=[xT_res[k]])
                  if b + 1 < NBLK:
                      cast_block(b + 1)
                  for m in range(4):
                      Pg = nmm(); Pu = nmm()
                      for k in range(8):
                          T(lambda e, Pg=Pg, k=k, m=m: e.matmul(
                              Pg.t[:, :], lhsT=wg_bf[p].t[:, k, :].rearrange("p (j m) -> p m j", m=4)[:, m, :], rhs=xT.t[:, k, :],
                              start=(k == 0), stop=(k == 7)), [wg_bf[p], xT_res[k]], [Pg], inc=(k == 7))
                      for k in range(8):
                          T(lambda e, Pu=Pu, k=k, m=m: e.matmul(
                              Pu.t[:, :], lhsT=wu_bf[p].t[:, k, :].rearrange("p (j m) -> p m j", m=4)[:, m, :], rhs=xT.t[:, k, :],
                              start=(k == 0), stop=(k == 7)), [wu_bf[p], xT_res[k]], [Pu], inc=(k == 7))
                      sl = sil[m % 2]
                      A(lambda e, Pg=Pg, sl=sl: e.activation(out=sl.t[:], in_=Pg.t[:, :], func=AF.Silu), [Pg], [sl])
                      V(lambda e, Pu=Pu, sl=sl, m=m: e.tensor_tensor(out=hT.t[:, m, :], in0=Pu.t[:, :], in1=sl.t[:], op=ALU.mult),
                        [Pu, sl], [], multi=[hT_res[m]])
                  for r in range(RT):
                      Py = [nmm(), nmm()]
                      for hf_ in range(2):
                          for m in range(4):
                              T(lambda e, r=r, hf_=hf_, m=m, Py=Py: e.matmul(
                                  Py[hf_].t[:, :], lhsT=hT.t[:, m, r * 128:(r + 1) * 128],
                                  rhs=wd_bf[p].t[:, m, hf_ * 512:(hf_ + 1) * 512], start=(m == 0), stop=(m == 3)),
                                [hT_res[m], wd_bf[p]], [Py[hf_]], inc=(m == 3))
                      yo_ = yo[stc["yo"] % 4]; stc["yo"] += 1
                      A(lambda e, yo_=yo_, Py=Py: e.activation(out=yo_.t[:, 0:512], in_=Py[0].t[:, :], func=AF.Copy), [Py[0]], [], multi=[yo_])
                      V(lambda e, yo_=yo_, Py=Py: e.tensor_copy(out=yo_.t[:, 512:1024], in_=Py[1].t[:, :]), [Py[1]], [], multi=[yo_])
                      row0 = b * BLK + r * 128
                      DM(lambda e, yo_=yo_, row0=row0: e.dma_start(out=yb_d[row0:row0 + 128, :], in_=yo_.t[:]),
                         [yo_, yb_zero], [], multi=[yb_res])
                  if ctx is not None:
                      pend, S.defer = S.defer, None
                      with nc.sync.If_cmp(nu_reg["sp"], b, "IS_GT"):
                          for f_ in pend["sp"]:
                              f_()
                      with nc.sync.Else():
                          for (sem_, prev_) in S.defer_dma:
                              nc.sync.wait_ge(sem_, prev_)
                              nc.sync.sem_inc(sem_, 16)
                      for q_ in SKIP_ENGS:
                          if q_ == "sp":
                              continue
                          eng_ = S.eng[q_]
                          with eng_.If_cmp(nu_reg[q_], b, "IS_GT"):
                              for f_ in pend[q_]:
                                  f_()
                          n_inc = S.ccnt[q_] - n0[q_]
                          assert n0[q_] // ROT == (S.ccnt[q_] - 1) // ROT and n0[q_] > 0
                          sem_ = S.csem[q_][n0[q_] // ROT]
                          with eng_.Else():
                              eng_.wait_ge(sem_, n0[q_] - (n0[q_] // ROT) * ROT)
                              for i_ in range(n_inc):
                                  eng_.sem_inc(sem_, 1)
                      for k_ in [k_ for k_ in S.waited if k_[0] in SKIP_ENGS]:
                          if k_ in snap:
                              S.waited[k_] = snap[k_]
                          else:
                              del S.waited[k_]
              S.barrier()

        with ExitStack() as esD:
          if stop is None:
              y1 = [sb(esD, f"y1_{i}", [128, D], BF16) for i in range(4)]
              ob = [sb(esD, f"ob_{i}", [128, D], F32) for i in range(4)]
              y2 = [sb(esD, f"y2_{i}", [128, D], BF16) for i in range(4)]
              xr = [sb(esD, f"xr_{i}", [128, D], F32) for i in range(4)]
              ssd = [sb(esD, f"ssd{i}", [128, 1], F32) for i in range(4)]
              lnd = [sb(esD, f"lnd{i}", [128, 1], F32) for i in range(4)]
              rsd = [sb(esD, f"rsd{i}", [128, 1], F32) for i in range(4)]
              junkd = esD.enter_context(nc.sbuf_tensor("s_junkd", [128, 1024], BF16))
              out_res = Res("out")
              fnw = sb(esD, "fnw", [128, D], F32)
              DM(lambda e: e.dma_start(out=fnw.t[:], in_=fnw_d), [], [fnw])
              fnw_bc = fnw.t[:, :]
              ND, PF = 4, 3

              def fetch(t):
                  i = t % ND
                  DG(lambda e: e.indirect_dma_start(
                      out=y1[i].t[:, :], out_offset=None, in_=yb_d[:, :],
                      in_offset=bass.IndirectOffsetOnAxis(ap=dest.t[:, t, 0:1], axis=0),
                      bounds_check=bc_reg, oob_is_err=False), [dest, yb_res], [y1[i]])
                  DG(lambda e: e.indirect_dma_start(
                      out=y2[i].t[:, :], out_offset=None, in_=yb_d[:, :],
                      in_offset=bass.IndirectOffsetOnAxis(ap=dest.t[:, t, 1:2], axis=0),
                      bounds_check=bc_reg, oob_is_err=False), [dest, yb_res], [y2[i]])
                  DM(lambda e: e.dma_start(out=xr[i].t[:], in_=x1_d[t * 128:(t + 1) * 128, :]), [x1_res[t]], [xr[i]])

              def combine(t):
                  i = t % ND
                  V(lambda e: e.scalar_tensor_tensor(out=xr[i].t[:], in0=y1[i].t[:], scalar=gates.t[:, t, 0:1],
                                                     in1=xr[i].t[:], op0=ALU.mult, op1=ALU.add),
                    [y1[i], gates, xr[i]], [xr[i]])
                  V(lambda e: e.scalar_tensor_tensor(out=xr[i].t[:], in0=y2[i].t[:], scalar=gates.t[:, t, 1:2],
                                                     in1=xr[i].t[:], op0=ALU.mult, op1=ALU.add),
                    [y2[i], gates, xr[i]], [xr[i]])
                  A(lambda e: e.activation(out=junkd[:, :], in_=xr[i].t[:], func=AF.Square, accum_out=ssd[i].t[:]),
                    [xr[i]], [ssd[i]])
                  A(lambda e: e.activation(out=lnd[i].t[:], in_=ssd[i].t[:], func=AF.Ln, scale=1.0 / D, bias=EPS),
                    [ssd[i]], [lnd[i]])
                  A(lambda e: e.activation(out=rsd[i].t[:], in_=lnd[i].t[:], func=AF.Exp, scale=-0.5), [lnd[i]], [rsd[i]])
                  V(lambda e: e.scalar_tensor_tensor(out=ob[i].t[:], in0=xr[i].t[:], scalar=rsd[i].t[:, 0:1],
                                                     in1=fnw_bc, op0=ALU.mult, op1=ALU.mult),
                    [xr[i], rsd[i], fnw], [ob[i]])
                  DM(lambda e: e.dma_start(out=out_d[t * 128:(t + 1) * 128, :], in_=ob[i].t[:]),
                     [ob[i]], [], multi=[out_res])

              for t in range(min(PF, NT)):
                  fetch(t)
              for t in range(NT):
                  if t + PF < NT:
                      fetch(t + PF)
                  combine(t)
              S.wait_all("sp", [out_res])
        print(f"[build] instructions={S.n_ins} waits={S.n_wait}")
    return nc


def _consts():
    ident = np.eye(128, dtype=np.float32)
    tp = np.arange(128)
    U = (tp[:, None] < tp[None, :]).astype(np.float32)
    ones = np.ones((128, 128), np.float32)
    onesm = np.full((128, 128), 1.0 / 512, np.float32)
    maskcur = np.where(tp[None, :] >= tp[:, None], 0.0, NEG).astype(np.float32)
    maskprev = np.where(tp[None, :] < tp[:, None], 0.0, NEG).astype(np.float32)
    c = np.stack([ident, U, ones, onesm, maskcur, maskprev], axis=1)
    return c.reshape(128, 6 * 128).astype(ml_dtypes.bfloat16)


def _fm(v, nchunk):
    return np.ascontiguousarray(np.asarray(v, np.float32).reshape(nchunk, 128).T)


def prepare_inputs(inputs, nseq, n_cores):
    f = lambda k: np.asarray(inputs[k])
    x = f("x").astype(np.float32, copy=False)
    pos = f("positions")
    L = 0
    vfm = np.zeros((128, VF), np.float32)
    vfm[:, 0:8] = _fm(f("attn_norm_w")[L], 8)
    vfm[:, 8:16] = _fm(f("ffn_norm_w")[L], 8)
    vfm[:, 16:24] = _fm(np.concatenate([f("attn_out_norm_w")[L], f("conv_out_norm_w")[L]]), 8)
    vfm[:, 24:28] = _fm(f("conv_dw_b")[L], 4)
    vfm[:, 28:32] = _fm(f("conv_ln_w")[L], 4)
    vfm[:, 32:36] = _fm(f("conv_ln_b")[L], 4)
    dww = f("conv_dw_w")[L]
    vfm[:, 36:160] = dww.reshape(KTAPS, 4, 128).transpose(2, 1, 0).reshape(128, 4 * KTAPS)
    vfm[:, 160] = np.arange(128, dtype=np.float32)
    half = 8
    invf = np.power(np.float32(THETA), -np.arange(half, dtype=np.float32) * np.float32(2.0) / np.float32(16)).astype(np.float32)
    vbc1 = np.concatenate([f("ffn_norm_w")[L],
                           f("router_group_b")[L], f("router_expert_b")[L],
                           f("attn_sinks")[L], invf,
                           np.arange(NE).astype(np.float32),
                           (np.arange(64) * BLK).astype(np.float32)]).astype(np.float32)
    vbc = np.ascontiguousarray(np.broadcast_to(vbc1[None, :], (128, VB)))
    wr = np.ascontiguousarray(np.concatenate([f("router_group_w")[L], f("router_expert_w")[L]], axis=1))
    cbf = _consts()
    w_in0 = f("w_in")[L]
    qcols = np.concatenate([np.arange(h * 64, (h + 1) * 64) for h in (0, 4, 1, 5, 2, 6, 3, 7)])
    w_in_p = np.ascontiguousarray(np.concatenate([w_in0[:, qcols], w_in0[:, 512:]], axis=1))
    shared = {
        "w_in": w_in_p, "w_out": np.ascontiguousarray(f("w_out")[L]), "w_r": wr,
        "w_gate": np.ascontiguousarray(f("w_gate")[L]), "w_up": np.ascontiguousarray(f("w_up")[L]),
        "w_down": np.ascontiguousarray(f("w_down")[L]), "vfm": vfm, "vbc": vbc, "cbf": cbf,
        "fnw": np.ascontiguousarray(np.broadcast_to(f("final_norm_w").astype(np.float32)[None, :], (128, D))),
    }
    in_maps = []
    for c in range(n_cores):
        xs = x[c * nseq:(c + 1) * nseq].reshape(nseq * SEQ, D)
        p = pos[c * nseq:(c + 1) * nseq].reshape(nseq * NT_SEQ, 128).T
        m = dict(shared)
        m["x"] = np.ascontiguousarray(xs)
        m["pos"] = np.ascontiguousarray(p.astype(np.int32))
        in_maps.append(m)
    return in_maps


def kernel(**inputs):
    B = inputs["x"].shape[0]
    nseq = B // N_CORES
    nc = build_program(nseq)
    in_maps = prepare_inputs(inputs, nseq, N_CORES)
    res = run_bass_kernel_spmd(nc, in_maps, core_ids=list(range(N_CORES)))
    outs = [np.asarray(r["out"]).reshape(nseq, SEQ, D) for r in res.results]
    return np.concatenate(outs, axis=0).astype(np.float32, copy=False)
```

```python
import os
import numpy as np
import ml_dtypes
from contextlib import ExitStack

import concourse.bass as bass
import concourse.mybir as mybir
from concourse.bass_utils import run_bass_kernel_spmd

F32 = mybir.dt.float32
BF16 = mybir.dt.bfloat16
I32 = mybir.dt.int32
AF = mybir.ActivationFunctionType
ALU = mybir.AluOpType
AX = mybir.AxisListType

N_CORES = 8
SEQ = 2048
D = 1024
NCOL = 1792
NE = 32
DE = 512
NT_SEQ = SEQ // 128
GT = 2
GW = GT * 128
HALO = 30
KTAPS = 31
BLK = 512
EPS = 1e-5
ROT = 30000
NDMASEM = 40
THETA = 500000.0
NEG = -30000.0

VF = 161
VB = 1024 + 36 + 8 + 8 + 32 + 64


class _Stop(Exception):
    pass


class Res:
    __slots__ = ("name", "w", "r", "excl")

    def __init__(self, name, excl=False):
        self.name = name
        self.w = {}
        self.r = {}
        self.excl = excl


class Buf:
    def __init__(self, t, name):
        self.t = t
        self.res = Res(name)


def _res(x):
    return x.res if isinstance(x, Buf) else x


class Sched:
    def __init__(self, nc, es):
        self.nc = nc
        self.es = es
        self.eng = {"pe": nc.tensor, "act": nc.scalar, "dve": nc.vector,
                    "pool": nc.gpsimd, "sp": nc.sync}
        self.csem = {e: [] for e in ("pe", "act", "dve", "pool")}
        self.ccnt = {e: 0 for e in ("pe", "act", "dve", "pool")}
        self.waited = {}
        self.dsem = {"sp": [], "pool": []}
        self.dcnt = {"sp": [], "pool": []}
        self.drr = {"sp": 0, "pool": 0}
        self.dmax = {"sp": 28, "pool": 16}
        self.n_wait = 0
        self.n_ins = 0
        self.defer = None
        self.defer_dma = []

    def _cur_event(self, e):
        n = self.ccnt[e]
        idx = n // ROT
        while len(self.csem[e]) <= idx:
            self.csem[e].append(self.es.enter_context(
                self.nc.semaphore(f"c_{e}_{len(self.csem[e])}")))
        return (self.csem[e][idx], (n % ROT) + 1, e)

    def _wait(self, e, ev):
        sem, val, src = ev
        if src == "pe" and e == "pe":
            return
        key = (e, sem.name)
        if self.waited.get(key, 0) >= val:
            return
        if self.defer is not None and e in self.defer:
            self.defer[e].append(lambda: self.eng[e].wait_ge(sem, val))
        else:
            self.eng[e].wait_ge(sem, val)
        self.waited[key] = val
        self.n_wait += 1

    @staticmethod
    def _merge(d, ev):
        sem, val, src = ev
        old = d.get(sem.name)
        if old is None or old[1] < val:
            d[sem.name] = ev

    def _deps(self, e, reads, writes, multi):
        for r in reads:
            rs = _res(r)
            for ev in list(rs.w.values()):
                self._wait(e, ev)
            if rs.excl:
                for ev in list(rs.r.values()):
                    if ev[2] != e:
                        self._wait(e, ev)
        for w in list(writes) + list(multi):
            rs = _res(w)
            for ev in list(rs.r.values()):
                self._wait(e, ev)
        for w in writes:
            for ev in list(_res(w).w.values()):
                self._wait(e, ev)

    def _commit(self, ev, reads, writes, multi):
        for r in reads:
            self._merge(_res(r).r, ev)
        for w in writes:
            rs = _res(w)
            rs.w = {ev[0].name: ev}
            rs.r = {}
        for w in multi:
            rs = _res(w)
            self._merge(rs.w, ev)

    def op(self, e, fn, reads=(), writes=(), inc=True, multi=()):
        self._deps(e, reads, writes, multi)
        ev = self._cur_event(e)
        if self.defer is not None and e in self.defer:
            def emit(fn=fn, ev=ev, inc=inc, e=e):
                ins_ = fn(self.eng[e])
                if inc:
                    ins_.then_inc(ev[0], 1)
            self.defer[e].append(emit)
            ins = None
        else:
            ins = fn(self.eng[e])
            if inc:
                ins.then_inc(ev[0], 1)
        if inc:
            self.ccnt[e] += 1
        self._commit(ev, reads, writes, multi)
        self.n_ins += 1
        return ins

    def dma(self, q, fn, reads=(), writes=(), multi=()):
        self._deps(q, reads, writes, multi)
        dsem, dcnt = self.dsem[q], self.dcnt[q]
        if len(dsem) < self.dmax[q]:
            dsem.append(self.es.enter_context(self.nc.semaphore(f"d_{q}_{len(dsem)}")))
            dcnt.append(0)
            slot = len(dsem) - 1
        else:
            slot = self.drr[q] % self.dmax[q]
        self.drr[q] += 1
        sem = dsem[slot]
        if dcnt[slot] > 0:
            self._wait(q, (sem, dcnt[slot] * 16, "dma"))
        dcnt[slot] += 1
        ev = (sem, dcnt[slot] * 16, "dma")
        if self.defer is not None and q in self.defer:
            def emit(fn=fn, sem=sem, q=q):
                fn(self.eng[q]).then_inc(sem, 16)
            self.defer[q].append(emit)
            self.defer_dma.append((sem, (dcnt[slot] - 1) * 16))
            ins = None
        else:
            ins = fn(self.eng[q])
            ins.then_inc(sem, 16)
        self._commit(ev, reads, writes, multi)
        self.n_ins += 1
        return ins

    def barrier(self):
        evs = []
        for e in ("pe", "act", "dve", "pool"):
            n = self.ccnt[e]
            if n > 0:
                idx = (n - 1) // ROT
                evs.append((self.csem[e][idx], ((n - 1) % ROT) + 1, e))
        for q_ in ("sp", "pool"):
            for slot, sem in enumerate(self.dsem[q_]):
                if self.dcnt[q_][slot] > 0:
                    evs.append((sem, self.dcnt[q_][slot] * 16, "dma"))
        for q in ("pe", "act", "dve", "pool", "sp"):
            for ev in evs:
                sem, val, src = ev
                if src == q:
                    if q == "pe":
                        continue
                key = (q, sem.name)
                if self.waited.get(key, 0) >= val:
                    continue
                self.eng[q].wait_ge(sem, val)
                self.waited[key] = val
                self.n_wait += 1

    def wait_all(self, q, resources):
        for r in resources:
            rs = _res(r)
            for ev in list(rs.w.values()) + list(rs.r.values()):
                self._wait(q, ev)


def build_program(nseq, debug=False, stop=None):
    NT = nseq * NT_SEQ
    NG = NT // GT
    NTOK = NT * 128
    NBLK = (NTOK * 2) // BLK + NE
    NROWS = NBLK * BLK
    RT = BLK // 128

    nc = bass.Bass("TRN2", target_bir_lowering=False)
    dram_in = lambda n, s, dt: nc.dram_tensor(n, s, dt, kind="ExternalInput").ap()
    x_d = dram_in("x", [NTOK, D], F32)
    pos_d = dram_in("pos", [128, NT], I32)
    win_d = dram_in("w_in", [D, NCOL], F32)
    wout_d = dram_in("w_out", [D, D], F32)
    wr_d = dram_in("w_r", [D, 36], F32)
    wg_d = dram_in("w_gate", [NE, D, DE], F32)
    wu_d = dram_in("w_up", [NE, D, DE], F32)
    wd_d = dram_in("w_down", [NE, DE, D], F32)
    vfm_d = dram_in("vfm", [128, VF], F32)
    vbc_d = dram_in("vbc", [128, VB], F32)
    fnw_d = dram_in("fnw", [128, D], F32)
    cbf_d = dram_in("cbf", [128, 6 * 128], BF16)
    out_d = nc.dram_tensor("out", [NTOK, D], F32, kind="ExternalOutput").ap()
    x1_d = nc.dram_tensor("x1s", [NTOK, D], F32,
                          kind="ExternalOutput" if debug else "Internal").ap()
    xs_d = nc.dram_tensor("xs", [NROWS, D], BF16).ap()
    hf_d = nc.dram_tensor("hfd", [NTOK, D], BF16).ap()
    yb_d = nc.dram_tensor("yb", [NROWS, D], BF16).ap()
    if debug:
        dbg_d = nc.dram_tensor("dbg", [128, NT, 4], F32, kind="ExternalOutput").ap()

    with ExitStack() as es:
        S = Sched(nc, es)

        def sb(es_, name, shape, dt):
            return Buf(es_.enter_context(nc.sbuf_tensor("s_" + name, shape, dt)), name)

        def ps(es_, name, shape, dt):
            b = Buf(es_.enter_context(nc.psum_tensor("p_" + name, shape, dt)), name)
            b.res.excl = True
            return b

        V = lambda fn, r=(), w=(), **k: S.op("dve", fn, r, w, **k)
        A = lambda fn, r=(), w=(), **k: S.op("act", fn, r, w, **k)
        Gp = lambda fn, r=(), w=(), **k: S.op("pool", fn, r, w, **k)
        T = lambda fn, r=(), w=(), **k: S.op("pe", fn, r, w, **k)
        DM = lambda fn, r=(), w=(), **k: S.dma("sp", fn, r, w, **k)
        DG = lambda fn, r=(), w=(), **k: S.dma("pool", fn, r, w, **k)

        bc_reg = nc.gpsimd.alloc_register("bc_rows")
        nc.gpsimd.reg_mov(bc_reg, NROWS - 1)
        bcw_reg = nc.gpsimd.alloc_register("bc_wrows")
        nc.gpsimd.reg_mov(bcw_reg, NE * 128 - 1)

        vfm = sb(es, "vfm", [128, VF], F32)
        vbc = sb(es, "vbc", [128, VB], F32)
        cbf = sb(es, "cbf", [128, 6, 128], BF16)
        dest = sb(es, "dest", [128, NT, 2], I32)
        gates = sb(es, "gates", [128, NT, 2], F32)
        exf = sb(es, "exf", [128, NT, 2], F32)
        rkf = sb(es, "rkf", [128, NT, 2], F32)
        cnt = sb(es, "cnt", [128, 32], F32)
        widx = sb(es, "widx", [128, 64], I32)
        nused_i = sb(es, "nused_i", [128, 1], I32)
        hf_res = [Res(f"hfd{t}") for t in range(NT)]
        x1_res = [Res(f"x1d{t}") for t in range(NT)]
        xs_res = Res("xs")
        xs_zero = Res("xs_zero")
        yb_zero = Res("yb_zero")
        yb_res = Res("yb")

        DM(lambda e: e.dma_start(out=vfm.t[:], in_=vfm_d), [], [vfm])
        DM(lambda e: e.dma_start(out=vbc.t[:], in_=vbc_d), [], [vbc])
        DM(lambda e: e.dma_start(out=cbf.t[:].rearrange("p a b -> p (a b)"), in_=cbf_d), [], [cbf])
        ident = cbf.t[:, 0, :]
        Utri = cbf.t[:, 1, :]
        ones_b = cbf.t[:, 2, :]
        onesm = cbf.t[:, 3, :]
        maskcur = cbf.t[:, 4, :]
        maskprev = cbf.t[:, 5, :]
        ffw_bc = vbc.t[:, 0:1024]
        rbias = vbc.t[:, 1024:1060]
        sinks_bc = vbc.t[:, 1060:1068]
        invf_bc = vbc.t[:, 1068:1076]
        iota_bc = vbc.t[:, 1076:1108]
        bstart_bc = vbc.t[:, 1108:1172]

        with ExitStack() as esA:
            w_in_bf = sb(esA, "w_in_bf", [128, 8, NCOL], BF16)
            w_in_res = [Res(f"w_in{k}") for k in range(8)]
            w_out_bf = sb(esA, "w_out_bf", [128, 8, D], BF16)
            w_out_res = [Res(f"w_out{k}") for k in range(8)]
            wr_bf = sb(esA, "wr_bf", [128, 8, 36], BF16)
            diag = sb(esA, "diag", [128, 4, KTAPS, 128], BF16)
            diag_res = [Res(f"diag{c}") for c in range(4)]
            ropeC = sb(esA, "ropeC", [128, NT, 16], F32)
            ropeS = sb(esA, "ropeS", [128, NT, 16], F32)
            esink = sb(esA, "esink", [128, 8], F32)
            nln = sb(esA, "nln", [128, 8], F32)
            junk = esA.enter_context(nc.sbuf_tensor("s_junk", [128, 1024], BF16))

            tpb = [ps(esA, f"tpb{i}", [128, 1024], BF16) for i in range(3)]
            mm = [ps(esA, f"mm{i}", [128, 512], F32) for i in range(5)]
            st_ = {"mm": 0, "tp": 0}

            def next_mm():
                b = mm[st_["mm"] % 5]
                st_["mm"] += 1
                return b

            def next_tp():
                b = tpb[st_["tp"] % 3]
                st_["tp"] += 1
                return b

            with ExitStack() as es0:
                stg = [sb(es0, f"stg{i}", [128, NCOL], F32) for i in range(2)]
                ident_f = sb(es0, "ident_f", [128, 128], F32)
                posf = sb(es0, "posf", [128, NT], F32)
                posi = sb(es0, "posi", [128, NT], I32)
                ang = sb(es0, "ang", [128, NT, 16], F32)
                kf = sb(es0, "kf", [128, NT, 16], F32)
                ki = sb(es0, "ki", [128, NT, 16], I32)
                mk = sb(es0, "mk", [128, NT, 16], F32)
                trg = sb(es0, "trg", [128, NT, 16], F32)

                DM(lambda e: e.dma_start(out=posi.t[:], in_=pos_d), [], [posi])
                V(lambda e: e.tensor_copy(out=posf.t[:], in_=posi.t[:]), [posi], [posf])
                V(lambda e: e.tensor_tensor(out=ang.t[:, :, 0:8],
                                            in0=posf.t[:].unsqueeze(2).to_broadcast([128, NT, 8]),
                                            in1=invf_bc.unsqueeze(1).to_broadcast([128, NT, 8]),
                                            op=ALU.mult), [posf, vbc], [ang])
                V(lambda e: e.tensor_scalar(out=ang.t[:, :, 8:16], in0=ang.t[:, :, 0:8],
                                            scalar1=float(np.pi / 2), scalar2=None, op0=ALU.add),
                  [ang], [ang])
                V(lambda e: e.tensor_scalar(out=kf.t[:], in0=ang.t[:], scalar1=float(1.0 / (2 * np.pi)),
                                            scalar2=None, op0=ALU.mult), [ang], [kf])
                V(lambda e: e.tensor_copy(out=ki.t[:], in_=kf.t[:]), [kf], [ki])
                V(lambda e: e.tensor_copy(out=kf.t[:], in_=ki.t[:]), [ki], [kf])
                V(lambda e: e.scalar_tensor_tensor(out=ang.t[:], in0=kf.t[:], scalar=float(-2 * np.pi),
                                                   in1=ang.t[:], op0=ALU.mult, op1=ALU.add),
                  [kf, ang], [ang])
                V(lambda e: e.tensor_single_scalar(out=mk.t[:], in_=ang.t[:], scalar=float(np.pi),
                                                   op=ALU.is_gt), [ang], [mk])
                V(lambda e: e.scalar_tensor_tensor(out=ang.t[:], in0=mk.t[:], scalar=float(-2 * np.pi),
                                                   in1=ang.t[:], op0=ALU.mult, op1=ALU.add),
                  [mk, ang], [ang])
                V(lambda e: e.tensor_single_scalar(out=mk.t[:], in_=ang.t[:], scalar=float(-np.pi),
                                                   op=ALU.is_lt), [ang], [mk])
                V(lambda e: e.scalar_tensor_tensor(out=ang.t[:], in0=mk.t[:], scalar=float(2 * np.pi),
                                                   in1=ang.t[:], op0=ALU.mult, op1=ALU.add),
                  [mk, ang], [ang])
                V(lambda e: e.tensor_scalar(out=ang.t[:], in0=ang.t[:], scalar1=float(np.pi), scalar2=float(-np.pi),
                                            op0=ALU.min, op1=ALU.max), [ang], [ang])
                A(lambda e: e.activation(out=trg.t[:], in_=ang.t[:], func=AF.Sin), [ang], [trg])
                V(lambda e: e.tensor_copy(out=ropeC.t[:, :, 0:8], in_=trg.t[:, :, 8:16]), [trg], [ropeC])
                V(lambda e: e.tensor_copy(out=ropeC.t[:, :, 8:16], in_=trg.t[:, :, 8:16]), [trg], [ropeC])
                V(lambda e: e.tensor_scalar(out=ropeS.t[:, :, 0:8], in0=trg.t[:, :, 0:8], scalar1=-1.0,
                                            scalar2=None, op0=ALU.mult), [trg], [ropeS])
                V(lambda e: e.tensor_copy(out=ropeS.t[:, :, 8:16], in_=trg.t[:, :, 0:8]), [trg], [ropeS])

                A(lambda e: e.activation(out=esink.t[:], in_=sinks_bc, func=AF.Exp), [vbc], [esink])
                V(lambda e: e.tensor_scalar(out=nln.t[:], in0=vfm.t[:, 28:36], scalar1=-1.0, scalar2=None,
                                            op0=ALU.mult), [vfm], [nln])
                V(lambda e: e.memset(cnt.t[:], 0.0), [], [cnt])
                V(lambda e: e.tensor_copy(out=ident_f.t[:], in_=ident), [cbf], [ident_f])

                cast_engs = ["act", "dve", "act"]
                ci = 0
                for k in range(8):
                    s_ = stg[k % 2]
                    DM(lambda e, s_=s_, k=k: e.dma_start(out=s_.t[:], in_=win_d[k * 128:(k + 1) * 128, :]), [], [s_])
                    eng = cast_engs[ci % 3]; ci += 1
                    if eng == "act":
                        A(lambda e, s_=s_, k=k: e.activation(out=w_in_bf.t[:, k, :], in_=s_.t[:], func=AF.Copy,
                                                             scale=vfm.t[:, k:k + 1]), [s_, vfm], [w_in_res[k]])
                    else:
                        S.op(eng, lambda e, s_=s_, k=k: e.tensor_scalar(out=w_in_bf.t[:, k, :], in0=s_.t[:],
                                                                         scalar1=vfm.t[:, k:k + 1], scalar2=None,
                                                                         op0=ALU.mult), [s_, vfm], [w_in_res[k]])
                for k in range(8):
                    s_ = stg[k % 2]
                    DM(lambda e, s_=s_, k=k: e.dma_start(out=s_.t[:, 0:D], in_=wout_d[k * 128:(k + 1) * 128, :]), [], [s_])
                    eng = cast_engs[ci % 3]; ci += 1
                    if eng == "act":
                        A(lambda e, s_=s_, k=k: e.activation(out=w_out_bf.t[:, k, :], in_=s_.t[:, 0:D], func=AF.Copy,
                                                             scale=vfm.t[:, 16 + k:17 + k]), [s_, vfm], [w_out_res[k]])
                    else:
                        S.op(eng, lambda e, s_=s_, k=k: e.tensor_scalar(out=w_out_bf.t[:, k, :], in0=s_.t[:, 0:D],
                                                                         scalar1=vfm.t[:, 16 + k:17 + k], scalar2=None,
                                                                         op0=ALU.mult), [s_, vfm], [w_out_res[k]])
                s_ = stg[0]
                DM(lambda e: e.dma_start(out=stg[0].t[:, 0:8 * 36].rearrange("p (k c) -> p k c", k=8),
                                         in_=wr_d.rearrange("(k p) c -> p k c", p=128)), [], [stg[0]])
                V(lambda e: e.tensor_copy(out=wr_bf.t[:], in_=stg[0].t[:, 0:8 * 36].rearrange("p (k c) -> p k c", k=8)),
                  [stg[0]], [wr_bf])
                di = 0
                for c in range(4):
                    for j in range(KTAPS):
                        di += 1
                        col = 36 + c * KTAPS + j
                        if di % 2 == 0:
                            A(lambda e, c=c, j=j, col=col: e.activation(out=diag.t[:, c, j, :], in_=ident_f.t[:], func=AF.Copy,
                                                                         scale=vfm.t[:, col:col + 1]), [ident_f, vfm], [],
                              multi=[diag_res[c]])
                        else:
                            V(lambda e, c=c, j=j, col=col: e.tensor_scalar(
                                out=diag.t[:, c, j, :], in0=ident_f.t[:], scalar1=vfm.t[:, col:col + 1],
                                scalar2=None, op0=ALU.mult), [ident_f, vfm], [], multi=[diag_res[c]])
                S.barrier()

            substop = None
            if stop and stop.startswith('A') and ':' in stop:
                substop = int(stop.split(':')[1])
                stop = stop.split(':')[0]
            NGrun = NG if not (stop and stop.startswith('A')) or stop == 'A' else int(stop[1:])

            def chk(k_):
                if substop == k_:
                    raise _Stop()
            xsb = [[sb(esA, f"xsb{p}{j}", [128, D], F32) for j in range(GT)] for p in range(3)]
            ssx = sb(esA, "ssx", [128, GT], F32)
            lnx = sb(esA, "lnx", [128, GT], F32)
            rstx = sb(esA, "rstx", [128, GT], F32)
            xsbf = [sb(esA, f"xsbf{j}", [128, D], BF16) for j in range(GT)]
            hnT = [sb(esA, "hnT0", [128, 8, GW], BF16)] * 2
            qsb = [sb(esA, f"qsb{j}", [128, 512], BF16) for j in range(GT)]
            ksb = [sb(esA, f"ksb{j}", [128, 128], BF16) for j in range(GT)]
            rt1 = [sb(esA, f"rt1{j}", [128, 10, 16], F32) for j in range(GT)]
            rt2 = [sb(esA, f"rt2{j}", [128, 10, 16], F32) for j in range(GT)]
            qTs = [[sb(esA, f"qT{p}{j}", [128, 4, 128], BF16) for j in range(GT)] for p in range(2)]
            kTb = [sb(esA, f"kT{i}", [128, 128], BF16) for i in range(5)]
            vaug = [sb(esA, f"vaug{i}", [128, 2, 65], BF16) for i in range(5)]
            Esb = [sb(esA, f"Esb{i}", [128, 8, 2, 128], BF16) for i in range(2)]
            den = sb(esA, "den", [128, 8], F32)
            rden = sb(esA, "rden", [128, 8], F32)
            attn32 = [sb(esA, f"attn32{j}", [128, 512], F32) for j in range(GT)]
            ssa = sb(esA, "ssa", [128, GT], F32)
            lna = sb(esA, "lna", [128, GT], F32)
            rsta = sb(esA, "rsta", [128, GT], F32)
            attnbf = [sb(esA, f"attnbf{j}", [128, 512], BF16) for j in range(GT)]
            mixTs = [sb(esA, f"mixT{p}", [128, 8, GW], BF16) for p in range(2)]
            mixT_a = [[Res(f"mixTa{p}{j}") for j in range(GT)] for p in range(2)]
            mixT_c = [Res(f"mixTc{p}") for p in range(2)]
            ubufs = [sb(esA, f"ubuf{p}", [128, 4, HALO + GW], BF16) for p in range(2)]
            egb = [sb(esA, f"eg{i}", [128, GW], F32) for i in range(2)]
            v32 = sb(esA, "v32", [128, 4, GW], F32)
            v32_res = [Res(f"v32_{c}") for c in range(4)]
            vbf = sb(esA, "vbf", [128, 4, GW], BF16)
            vbf_res = [Res(f"vbf_{c}") for c in range(4)]
            sqbf = sb(esA, "sqbf", [128, 4, GW], BF16)
            sqbf_res = [Res(f"sqbf_{c}") for c in range(4)]
            mean_sb = sb(esA, "mean_sb", [128, GW], F32)
            m2_sb = sb(esA, "m2_sb", [128, GW], F32)
            rstd_sb = sb(esA, "rstd_sb", [128, GW], F32)
            r_sb = sb(esA, "r_sb", [128, GW], F32)
            ybuf = [sb(esA, f"ybuf{i}", [128, GW], F32) for i in range(2)]
            x1sb = [sb(esA, f"x1sb{j}", [128, D], F32) for j in range(GT)]
            ss1 = sb(esA, "ss1", [128, GT], F32)
            ln1 = sb(esA, "ln1", [128, GT], F32)
            rst1 = sb(esA, "rst1", [128, GT], F32)
            hfbf = [[sb(esA, f"hfbf{p}{j}", [128, D], BF16) for j in range(GT)] for p in range(2)]
            hfT = [sb(esA, f"hfT{j}", [128, 8, 128], BF16) for j in range(GT)]
            lgs = [sb(esA, f"lg{p}", [128, GT, 36], F32) for p in range(2)]
            gmax = sb(esA, "gmax", [128, GT], F32)
            ohg = sb(esA, "ohg", [128, GT, 4], F32)
            gsh = sb(esA, "gsh", [128, GT, 4], F32)
            gsum = sb(esA, "gsum", [128, GT], F32)
            gp = sb(esA, "gp", [128, GT], F32)
            tmp48 = sb(esA, "tmp48", [128, GT, 4, 8], F32)
            esel = sb(esA, "esel", [128, GT, 8], F32)
            m1 = sb(esA, "m1", [128, GT], F32)
            oh1 = sb(esA, "oh1", [128, GT, 8], F32)
            em = sb(esA, "em", [128, GT, 8], F32)
            m2 = sb(esA, "m2", [128, GT], F32)
            oh2 = sb(esA, "oh2", [128, GT, 8], F32)
            d21 = sb(esA, "d21", [128, GT], F32)
            e21 = sb(esA, "e21", [128, GT], F32)
            dn = sb(esA, "dn", [128, GT], F32)
            g1 = sb(esA, "g1", [128, GT], F32)
            g2 = sb(esA, "g2", [128, GT], F32)
            oh1fs = [sb(esA, f"oh1f{p}", [128, GT, 32], F32) for p in range(2)]
            oh2fs = [sb(esA, f"oh2f{p}", [128, GT, 32], F32) for p in range(2)]
            ohbs = [sb(esA, f"ohb{p}", [128, GT, 32], BF16) for p in range(2)]
            rk = sb(esA, "rk", [128, GT, 32], F32)
            t32 = sb(esA, "t32", [128, GT, 32], F32)

            zt = sb(esA, "zt", [128, D], BF16)
            V(lambda e: e.memset(zt.t[:], 0.0), [], [zt])
            zf_state = {"r": 0}

            def zero_fill(nchunks):
                for _ in range(nchunks):
                    r0 = zf_state["r"]
                    if r0 >= NROWS:
                        return
                    DG(lambda e, r0=r0: e.dma_start(out=xs_d[r0:r0 + 128, :], in_=zt.t[:]), [zt], [], multi=[xs_zero])
                    zf_state["r"] = r0 + 128
                    if r0 >= ((NTOK * 2 + BLK - 1) // BLK) * BLK:
                        DG(lambda e, r0=r0: e.dma_start(out=yb_d[r0:r0 + 128, :], in_=zt.t[:]), [zt], [], multi=[yb_zero])
            for p_ in range(2):
                V(lambda e, p_=p_: e.memset(ubufs[p_].t[:], 0.0), [], [ubufs[p_]])
            for i in range(5):
                V(lambda e, i=i: e.memset(vaug[i].t[:], 1.0), [], [vaug[i]])

            def load_x(G):
                p = G % 3
                for j in range(GT):
                    t = G * GT + j
                    DM(lambda e, p=p, j=j, t=t: e.dma_start(out=xsb[p][j].t[:], in_=x_d[t * 128:(t + 1) * 128, :]),
                       [], [xsb[p][j]])

            def stage_F(G):
                par = G % 2
                gs = G % (NT_SEQ // GT)
                load_x(G)
                xg = xsb[G % 3]
                ubuf = ubufs[par]
                ubuf_prev = ubufs[1 - par]
                for j in range(GT):
                    A(lambda e, j=j: e.activation(out=junk[:, :], in_=xg[j].t[:], func=AF.Square,
                                                  accum_out=ssx.t[:, j:j + 1]), [xg[j]], [ssx])
                A(lambda e: e.activation(out=lnx.t[:], in_=ssx.t[:], func=AF.Ln, scale=1.0 / D, bias=EPS),
                  [ssx], [lnx])
                A(lambda e: e.activation(out=rstx.t[:], in_=lnx.t[:], func=AF.Exp, scale=-0.5), [lnx], [rstx])
                for j in range(GT):
                    A(lambda e, j=j: e.activation(out=xsbf[j].t[:], in_=xg[j].t[:], func=AF.Copy, scale=rstx.t[:, j:j + 1]),
                      [xg[j], rstx], [xsbf[j]])
                yield
                hn = hnT[par]
                for half in range(2):
                    tp = next_tp()
                    for kk in range(4):
                        k = half * 4 + kk
                        for j in range(GT):
                            T(lambda e, tp=tp, kk=kk, j=j, k=k: e.transpose(
                                out=tp.t[:, kk * GW + j * 128: kk * GW + (j + 1) * 128],
                                in_=xsbf[j].t[:, k * 128:(k + 1) * 128], identity=ident),
                              [xsbf[j], cbf], [tp], inc=(kk == 3 and j == GT - 1))
                    fn = (lambda e, tp=tp, half=half: e.tensor_copy(
                        out=hn.t[:, half * 4:(half + 1) * 4, :].rearrange("p a b -> p (a b)"), in_=tp.t[:, :]))
                    if half == 0:
                        V(fn, [tp], [hn])
                    else:
                        A(lambda e, tp=tp, half=half: e.activation(
                            out=hn.t[:, half * 4:(half + 1) * 4, :].rearrange("p a b -> p (a b)"), in_=tp.t[:, :],
                            func=AF.Copy), [tp], [hn])
                yield
                Pq = [None] * GT
                slots = []
                for j in range(GT):
                    yield
                    t = G * GT + j
                    n = gs * GT + j
                    slot = t % 5
                    Pq[j] = next_mm()
                    Pkv = next_mm()
                    for k in range(8):
                        T(lambda e, j=j, k=k: e.matmul(Pq[j].t[:, :], lhsT=hn.t[:, k, j * 128:(j + 1) * 128],
                                                       rhs=w_in_bf.t[:, k, 0:512], start=(k == 0), stop=(k == 7)),
                          [hn, w_in_res[k]], [Pq[j]], inc=(k == 7))
                    for k in range(8):
                        T(lambda e, j=j, k=k, Pkv=Pkv: e.matmul(Pkv.t[:, 0:256],
                                                                 lhsT=hn.t[:, k, j * 128:(j + 1) * 128],
                                                                 rhs=w_in_bf.t[:, k, 512:768], start=(k == 0), stop=(k == 7)),
                          [hn, w_in_res[k]], [Pkv], inc=(k == 7))
                    slots.append(slot)
                    A(lambda e, j=j: e.activation(out=qsb[j].t[:], in_=Pq[j].t[:], func=AF.Copy), [Pq[j]], [qsb[j]])
                    q3 = Pq[j].t[:].rearrange("p (h d) -> p h d", h=8)
                    k3 = Pkv.t[:, 0:128].rearrange("p (h d) -> p h d", h=2)
                    Cb = lambda nh, t=t: ropeC.t[:, t, :].unsqueeze(1).to_broadcast([128, nh, 16])
                    Sa = lambda nh, t=t: ropeS.t[:, t, 0:8].unsqueeze(1).to_broadcast([128, nh, 8])
                    Sb = lambda nh, t=t: ropeS.t[:, t, 8:16].unsqueeze(1).to_broadcast([128, nh, 8])
                    V(lambda e, j=j, q3=q3, Cb=Cb: e.tensor_tensor(out=rt1[j].t[:, 0:8, :], in0=q3[:, :, 0:16], in1=Cb(8),
                                                                   op=ALU.mult), [Pq[j], ropeC], [rt1[j]])
                    V(lambda e, j=j, q3=q3, Sa=Sa: e.tensor_tensor(out=rt2[j].t[:, 0:8, 0:8], in0=q3[:, :, 8:16], in1=Sa(8),
                                                                   op=ALU.mult), [Pq[j], ropeS], [rt2[j]])
                    V(lambda e, j=j, q3=q3, Sb=Sb: e.tensor_tensor(out=rt2[j].t[:, 0:8, 8:16], in0=q3[:, :, 0:8], in1=Sb(8),
                                                                   op=ALU.mult), [Pq[j], ropeS], [rt2[j]])
                    V(lambda e, j=j, k3=k3, Cb=Cb: e.tensor_tensor(out=rt1[j].t[:, 8:10, :], in0=k3[:, :, 0:16], in1=Cb(2),
                                                                   op=ALU.mult), [Pkv, ropeC], [rt1[j]])
                    V(lambda e, j=j, k3=k3, Sa=Sa: e.tensor_tensor(out=rt2[j].t[:, 8:10, 0:8], in0=k3[:, :, 8:16], in1=Sa(2),
                                                                   op=ALU.mult), [Pkv, ropeS], [rt2[j]])
                    V(lambda e, j=j, k3=k3, Sb=Sb: e.tensor_tensor(out=rt2[j].t[:, 8:10, 8:16], in0=k3[:, :, 0:8], in1=Sb(2),
                                                                   op=ALU.mult), [Pkv, ropeS], [rt2[j]])
                    V(lambda e, j=j, Pkv=Pkv: e.tensor_copy(out=ksb[j].t[:], in_=Pkv.t[:, 0:128]), [Pkv], [ksb[j]])
                    V(lambda e, j=j, slot=slot: e.tensor_copy(
                        out=vaug[slot].t[:, :, 0:64],
                        in_=Pkv.t[:, 128:256].rearrange("p (h d) -> p h d", h=2)),
                      [Pkv], [vaug[slot]])
                    V(lambda e, j=j: e.tensor_tensor(
                        out=qsb[j].t[:].rearrange("p (h d) -> p h d", h=8)[:, :, 0:16],
                        in0=rt1[j].t[:, 0:8, :], in1=rt2[j].t[:, 0:8, :], op=ALU.add), [rt1[j], rt2[j]], [qsb[j]])
                    V(lambda e, j=j: e.tensor_tensor(
                        out=ksb[j].t[:].rearrange("p (h d) -> p h d", h=2)[:, :, 0:16],
                        in0=rt1[j].t[:, 8:10, :], in1=rt2[j].t[:, 8:10, :], op=ALU.add), [rt1[j], rt2[j]], [ksb[j]])
                    yield
                    yield
                    tp = next_tp()
                    for i in range(4):
                        T(lambda e, j=j, i=i, tp=tp: e.transpose(
                            out=tp.t[:, i * 128:(i + 1) * 128],
                            in_=qsb[j].t[:, i * 128:(i + 1) * 128],
                            identity=ident), [qsb[j], cbf], [tp], inc=False)
                    T(lambda e, j=j, tp=tp: e.transpose(out=tp.t[:, 512:640], in_=ksb[j].t[:], identity=ident),
                      [ksb[j], cbf], [tp])
                    V(lambda e, j=j, tp=tp: e.tensor_copy(out=qTs[par][j].t[:].rearrange("p a b -> p (a b)"), in_=tp.t[:, 0:512]),
                      [tp], [qTs[par][j]])
                    V(lambda e, slot=slot, tp=tp: e.tensor_copy(out=kTb[slot].t[:], in_=tp.t[:, 512:640]),
                      [tp], [kTb[slot]])
                yield
                if gs == 0:
                    V(lambda e: e.memset(ubuf.t[:, :, 0:HALO], 0.0), [], [ubuf])
                else:
                    V(lambda e: e.tensor_copy(out=ubuf.t[:, :, 0:HALO], in_=ubuf_prev.t[:, :, GW:GW + HALO]), [ubuf_prev], [ubuf])
                for c in range(4):
                    yield
                    Pag = next_mm()
                    for k in range(8):
                        T(lambda e, c=c, k=k, Pag=Pag: e.matmul(Pag.t[:, 0:GW], lhsT=w_in_bf.t[:, k, 768 + c * 128: 896 + c * 128],
                                                                 rhs=hn.t[:, k, :], start=(k == 0), stop=(k == 7)),
                          [hn, w_in_res[k]], [Pag], inc=False)
                    for k in range(8):
                        T(lambda e, c=c, k=k, Pag=Pag: e.matmul(Pag.t[:, GW:2 * GW], lhsT=w_in_bf.t[:, k, 1280 + c * 128: 1408 + c * 128],
                                                                 rhs=hn.t[:, k, :], start=(k == 0), stop=(k == 7)),
                          [hn, w_in_res[k]], [Pag], inc=(k == 7))
                    eg = egb[c % 2]
                    A(lambda e, Pag=Pag, eg=eg: e.activation(out=eg.t[:], in_=Pag.t[:, GW:2 * GW], func=AF.Exp, scale=-1.0),
                      [Pag], [eg])
                    A(lambda e, eg=eg: e.activation(out=eg.t[:], in_=eg.t[:], func=AF.Ln, bias=1.0, scale=1.0), [eg], [eg])
                    A(lambda e, eg=eg: e.activation(out=eg.t[:], in_=eg.t[:], func=AF.Exp, scale=-1.0), [eg], [eg])
                    V(lambda e, eg=eg, Pag=Pag, c=c: e.tensor_tensor(out=ubuf.t[:, c, HALO:HALO + GW], in0=Pag.t[:, 0:GW],
                                                                     in1=eg.t[:], op=ALU.mult), [Pag, eg, ubuf], [ubuf])

            def stage_Ma(G):
                par = G % 2
                gs = G % (NT_SEQ // GT)
                ubuf = ubufs[par]
                mixT = mixTs[par]
                for j in range(GT):
                    t = G * GT + j
                    n = gs * GT + j
                    slot = t % 5
                    pslot = (t - 1) % 5
                    Eb = Esb[t % 2]
                    for hp in range(4):
                        yield
                        STb = next_mm()
                        for hh in range(2):
                            h = 2 * hp + hh
                            kv = h // 4
                            i = h % 4
                            rows = slice(kv * 64, kv * 64 + 64)
                            base = hh * 256
                            if n > 0:
                                T(lambda e, STb=STb, rows=rows, i=i, j=j, base=base, pslot=pslot: e.matmul(
                                    STb.t[:, base: base + 128], lhsT=kTb[pslot].t[rows, :], rhs=qTs[par][j].t[rows, i, :],
                                    start=True, stop=False), [kTb[pslot], qTs[par][j]], [STb], inc=False)
                                T(lambda e, STb=STb, base=base: e.matmul(
                                    STb.t[:, base: base + 128], lhsT=ident, rhs=maskprev, start=False, stop=True),
                                  [cbf], [STb], inc=False)
                            T(lambda e, STb=STb, rows=rows, i=i, j=j, base=base, slot=slot: e.matmul(
                                STb.t[:, base + 128: base + 256], lhsT=kTb[slot].t[rows, :], rhs=qTs[par][j].t[rows, i, :],
                                start=True, stop=False), [kTb[slot], qTs[par][j]], [STb], inc=False)
                            T(lambda e, STb=STb, base=base: e.matmul(
                                STb.t[:, base + 128: base + 256], lhsT=ident, rhs=maskcur, start=False, stop=True),
                              [cbf], [STb], inc=(hh == 1))
                        if n > 0:
                            A(lambda e, STb=STb, Eb=Eb, hp=hp: e.activation(
                                out=Eb.t[:, 2 * hp:2 * hp + 2, :, :].rearrange("p a b c -> p (a b c)"), in_=STb.t[:, :],
                                func=AF.Exp, scale=0.125), [STb], [Eb])
                        else:
                            A(lambda e, STb=STb, Eb=Eb, hp=hp: e.activation(
                                out=Eb.t[:, 2 * hp:2 * hp + 2, 1, :],
                                in_=STb.t[:, :].rearrange("p (a b c) -> p a b c", a=2, b=2)[:, :, 1, :],
                                func=AF.Exp, scale=0.125), [STb], [Eb])
                    yield
                    yield
                    PO = [next_mm(), next_mm()]
                    for h in range(8):
                        kv = h // 4
                        ob = PO[h // 4]
                        oc = (h % 4) * 128
                        if n > 0:
                            T(lambda e, ob=ob, oc=oc, h=h, kv=kv, Eb=Eb, pslot=pslot: e.matmul(
                                ob.t[:, oc: oc + 65], lhsT=Eb.t[:, h, 0, :], rhs=vaug[pslot].t[:, kv, :],
                                start=True, stop=False), [Eb, vaug[pslot]], [ob], inc=False)
                        T(lambda e, ob=ob, oc=oc, h=h, kv=kv, Eb=Eb, slot=slot, n=n: e.matmul(
                            ob.t[:, oc: oc + 65], lhsT=Eb.t[:, h, 1, :], rhs=vaug[slot].t[:, kv, :],
                            start=(n == 0), stop=True), [Eb, vaug[slot]], [ob], inc=(h % 4 == 3))
                    for hb in range(2):
                        o3 = PO[hb].t[:].rearrange("p (h d) -> p h d", h=4)
                        V(lambda e, o3=o3, hb=hb: e.tensor_tensor(out=den.t[:, hb * 4:(hb + 1) * 4], in0=o3[:, :, 64],
                                                                   in1=esink.t[:, hb * 4:(hb + 1) * 4], op=ALU.add),
                          [PO[hb], esink], [den])
                    V(lambda e: e.reciprocal(out=rden.t[:], in_=den.t[:]), [den], [rden])
                    for hb in range(2):
                        o3 = PO[hb].t[:].rearrange("p (h d) -> p h d", h=4)
                        V(lambda e, o3=o3, hb=hb, j=j: e.tensor_tensor(
                            out=attn32[j].t[:, hb * 256:(hb + 1) * 256].rearrange("p (h d) -> p h d", h=4),
                            in0=o3[:, :, 0:64],
                            in1=rden.t[:, hb * 4:(hb + 1) * 4].unsqueeze(2).to_broadcast([128, 4, 64]),
                            op=ALU.mult), [PO[hb], rden], [attn32[j]])
                    A(lambda e, j=j: e.activation(out=junk[:, 0:512], in_=attn32[j].t[:], func=AF.Square,
                                                  accum_out=ssa.t[:, j:j + 1]), [attn32[j]], [ssa])
                yield
                A(lambda e: e.activation(out=lna.t[:], in_=ssa.t[:], func=AF.Ln, scale=1.0 / 512, bias=EPS), [ssa], [lna])
                A(lambda e: e.activation(out=rsta.t[:], in_=lna.t[:], func=AF.Exp, scale=-0.5), [lna], [rsta])
                for j in range(GT):
                    A(lambda e, j=j: e.activation(out=attnbf[j].t[:], in_=attn32[j].t[:], func=AF.Copy, scale=rsta.t[:, j:j + 1]),
                      [attn32[j], rsta], [attnbf[j]])
                for j in range(GT):
                    yield
                    tp = next_tp()
                    for i in range(4):
                        T(lambda e, j=j, i=i, tp=tp: e.transpose(out=tp.t[:, i * 128:(i + 1) * 128],
                                                                  in_=attnbf[j].t[:, i * 128:(i + 1) * 128], identity=ident),
                          [attnbf[j], cbf], [tp], inc=(i == 3))
                    V(lambda e, j=j, tp=tp: e.tensor_copy(
                        out=mixT.t[:, 0:4, j * 128:(j + 1) * 128],
                        in_=tp.t[:, 0:512].rearrange("p (a b) -> p a b", a=4)), [tp], [mixT_a[par][j]])

            def stage_Mc(G):
                par = G % 2
                gs = G % (NT_SEQ // GT)
                ubuf = ubufs[par]
                mixT = mixTs[par]
                for c in range(4):
                    yield
                    Pv = next_mm()
                    for jt in range(KTAPS):
                        T(lambda e, c=c, jt=jt, Pv=Pv: e.matmul(Pv.t[:, 0:GW], lhsT=diag.t[:, c, jt, :],
                                                                 rhs=ubuf.t[:, c, jt: jt + GW], start=(jt == 0),
                                                                 stop=(jt == KTAPS - 1)),
                          [diag_res[c], ubuf], [Pv], inc=(jt == KTAPS - 1))
                    A(lambda e, c=c, Pv=Pv: e.activation(out=v32.t[:, c, :], in_=Pv.t[:, 0:GW], func=AF.Identity,
                                                         bias=vfm.t[:, 24 + c:25 + c], scale=1.0), [Pv, vfm], [v32_res[c]])
                    A(lambda e, c=c, Pv=Pv: e.activation(out=vbf.t[:, c, :], in_=Pv.t[:, 0:GW], func=AF.Identity,
                                                         bias=vfm.t[:, 24 + c:25 + c], scale=1.0), [Pv, vfm], [vbf_res[c]])
                    A(lambda e, c=c, Pv=Pv: e.activation(out=sqbf.t[:, c, :], in_=Pv.t[:, 0:GW], func=AF.Square,
                                                         bias=vfm.t[:, 24 + c:25 + c], scale=1.0), [Pv, vfm], [sqbf_res[c]])
                yield
                yield
                Pst = next_mm()
                for c in range(4):
                    T(lambda e, c=c: e.matmul(Pst.t[:, 0:GW], lhsT=onesm, rhs=vbf.t[:, c, :], start=(c == 0), stop=(c == 3)),
                      [vbf_res[c], cbf], [Pst], inc=False)
                for c in range(4):
                    T(lambda e, c=c: e.matmul(Pst.t[:, GW:2 * GW], lhsT=onesm, rhs=sqbf.t[:, c, :], start=(c == 0), stop=(c == 3)),
                      [sqbf_res[c], cbf], [Pst], inc=(c == 3))
                V(lambda e: e.tensor_copy(out=mean_sb.t[:], in_=Pst.t[:, 0:GW]), [Pst], [mean_sb])
                A(lambda e: e.activation(out=m2_sb.t[:], in_=Pst.t[:, 0:GW], func=AF.Square), [Pst], [m2_sb])
                V(lambda e: e.tensor_tensor(out=rstd_sb.t[:], in0=Pst.t[:, GW:2 * GW], in1=m2_sb.t[:], op=ALU.subtract),
                  [Pst, m2_sb], [rstd_sb])
                A(lambda e: e.activation(out=rstd_sb.t[:], in_=rstd_sb.t[:], func=AF.Ln, bias=EPS, scale=1.0), [rstd_sb], [rstd_sb])
                A(lambda e: e.activation(out=rstd_sb.t[:], in_=rstd_sb.t[:], func=AF.Exp, scale=-0.5), [rstd_sb], [rstd_sb])
                yield
                for c in range(4):
                    yield
                    yb_ = ybuf[c % 2]
                    eg = egb[c % 2]
                    V(lambda e, c=c: e.tensor_tensor(out=v32.t[:, c, :], in0=v32.t[:, c, :], in1=mean_sb.t[:], op=ALU.subtract),
                      [v32_res[c], mean_sb], [v32_res[c]])
                    V(lambda e, c=c: e.tensor_tensor(out=v32.t[:, c, :], in0=v32.t[:, c, :], in1=rstd_sb.t[:], op=ALU.mult),
                      [v32_res[c], rstd_sb], [v32_res[c]])
                    V(lambda e, c=c, yb_=yb_: e.tensor_scalar(out=yb_.t[:], in0=v32.t[:, c, :], scalar1=vfm.t[:, 28 + c:29 + c],
                                                              scalar2=vfm.t[:, 32 + c:33 + c], op0=ALU.mult, op1=ALU.add),
                      [v32_res[c], vfm], [yb_])
                    A(lambda e, c=c, eg=eg: e.activation(out=eg.t[:], in_=v32.t[:, c, :], func=AF.Exp,
                                                         scale=nln.t[:, c:c + 1], bias=nln.t[:, 4 + c:5 + c]),
                      [v32_res[c], nln], [eg])
                    A(lambda e, eg=eg: e.activation(out=eg.t[:], in_=eg.t[:], func=AF.Ln, bias=1.0, scale=1.0), [eg], [eg])
                    A(lambda e, eg=eg: e.activation(out=eg.t[:], in_=eg.t[:], func=AF.Exp, scale=-1.0), [eg], [eg])
                    V(lambda e, c=c, eg=eg, yb_=yb_: e.tensor_tensor(out=v32.t[:, c, :], in0=yb_.t[:], in1=eg.t[:], op=ALU.mult),
                      [yb_, eg, v32_res[c]], [v32_res[c]])
                    A(lambda e, c=c: e.activation(out=sqbf.t[:, c, :], in_=v32.t[:, c, :], func=AF.Square),
                      [v32_res[c]], [sqbf_res[c]])
                yield
                yield
                yield
                Pms = next_mm()
                for c in range(4):
                    T(lambda e, c=c: e.matmul(Pms.t[:, 0:GW], lhsT=onesm, rhs=sqbf.t[:, c, :], start=(c == 0), stop=(c == 3)),
                      [sqbf_res[c], cbf], [Pms], inc=(c == 3))
                A(lambda e: e.activation(out=r_sb.t[:], in_=Pms.t[:, 0:GW], func=AF.Ln, bias=EPS, scale=1.0), [Pms], [r_sb])
                A(lambda e: e.activation(out=r_sb.t[:], in_=r_sb.t[:], func=AF.Exp, scale=-0.5), [r_sb], [r_sb])
                yield
                for c in range(4):
                    S.op("dve", lambda e, c=c: e.tensor_tensor(out=mixT.t[:, 4 + c, :], in0=v32.t[:, c, :], in1=r_sb.t[:], op=ALU.mult),
                         [v32_res[c], r_sb], [], multi=[mixT_c[par]])

            def stage_E(G):
                par = G % 2
                xg = xsb[G % 3]
                mixT = mixTs[par]
                lg = lgs[par]
                zero_fill((NROWS // 128 + NG - 1) // NG)
                for j in range(GT):
                    yield
                    t = G * GT + j
                    Po = [next_mm(), next_mm()]
                    for hf_ in range(2):
                        for k in range(8):
                            rd = [w_out_res[k], mixT_a[par][j] if k < 4 else mixT_c[par]]
                            T(lambda e, hf_=hf_, k=k, j=j: e.matmul(Po[hf_].t[:, :], lhsT=mixT.t[:, k, j * 128:(j + 1) * 128],
                                                                     rhs=w_out_bf.t[:, k, hf_ * 512:(hf_ + 1) * 512],
                                                                     start=(k == 0), stop=(k == 7)),
                              rd, [Po[hf_]], inc=(k == 7))
                    for hf_ in range(2):
                        V(lambda e, hf_=hf_, j=j: e.tensor_tensor(out=x1sb[j].t[:, hf_ * 512:(hf_ + 1) * 512],
                                                                   in0=Po[hf_].t[:, :], in1=xg[j].t[:, hf_ * 512:(hf_ + 1) * 512],
                                                                   op=ALU.add), [Po[hf_], xg[j]], [x1sb[j]])
                    DG(lambda e, j=j, t=t: e.dma_start(out=x1_d[t * 128:(t + 1) * 128, :], in_=x1sb[j].t[:]),
                       [x1sb[j]], [x1_res[t]])
                    A(lambda e, j=j: e.activation(out=junk[:, :], in_=x1sb[j].t[:], func=AF.Square,
                                                  accum_out=ss1.t[:, j:j + 1]), [x1sb[j]], [ss1])
                yield
                A(lambda e: e.activation(out=ln1.t[:], in_=ss1.t[:], func=AF.Ln, scale=1.0 / D, bias=EPS), [ss1], [ln1])
                A(lambda e: e.activation(out=rst1.t[:], in_=ln1.t[:], func=AF.Exp, scale=-0.5), [ln1], [rst1])
                for j in range(GT):
                    hb_ = hfbf[par][j]
                    V(lambda e, j=j, hb_=hb_: e.scalar_tensor_tensor(out=hb_.t[:], in0=x1sb[j].t[:], scalar=rst1.t[:, j:j + 1],
                                                                      in1=ffw_bc, op0=ALU.mult, op1=ALU.mult),
                      [x1sb[j], rst1, vbc], [hb_])
                for j in range(GT):
                    yield
                    yield
                    hb_ = hfbf[par][j]
                    tp = next_tp()
                    for k in range(8):
                        T(lambda e, k=k, tp=tp, hb_=hb_: e.transpose(out=tp.t[:, k * 128:(k + 1) * 128],
                                                                      in_=hb_.t[:, k * 128:(k + 1) * 128], identity=ident),
                          [hb_, cbf], [tp], inc=(k == 7))
                    A(lambda e, j=j, tp=tp: e.activation(out=hfT[j].t[:].rearrange("p a b -> p (a b)"), in_=tp.t[:, :],
                                                         func=AF.Copy), [tp], [hfT[j]])
                for j in range(GT):
                    yield
                    Pr = next_mm()
                    for k in range(8):
                        T(lambda e, j=j, k=k, Pr=Pr: e.matmul(Pr.t[:, 0:36], lhsT=hfT[j].t[:, k, :], rhs=wr_bf.t[:, k, :],
                                                               start=(k == 0), stop=(k == 7)), [hfT[j], wr_bf], [Pr], inc=(k == 7))
                    V(lambda e, j=j, Pr=Pr: e.tensor_tensor(out=lg.t[:, j, :], in0=Pr.t[:, 0:36], in1=rbias, op=ALU.add),
                      [Pr, vbc], [lg])
                yield
                t0 = G * GT
                for j in range(GT):
                    t = t0 + j
                    DG(lambda e, j=j, t=t: e.dma_start(out=hf_d[t * 128:(t + 1) * 128, :], in_=hfbf[par][j].t[:]),
                       [hfbf[par][j]], [hf_res[t]])

            def stage_R1(G):
                par = G % 2
                lg = lgs[par]
                oh1f, oh2f, ohb = oh1fs[par], oh2fs[par], ohbs[par]
                R3 = lambda ap, n_: ap.unsqueeze(2).to_broadcast([128, GT, n_])
                V(lambda e: e.tensor_reduce(out=gmax.t[:], in_=lg.t[:, :, 0:4], axis=AX.X, op=ALU.max), [lg], [gmax])
                V(lambda e: e.tensor_tensor(out=ohg.t[:], in0=lg.t[:, :, 0:4], in1=R3(gmax.t[:], 4), op=ALU.is_equal),
                  [lg, gmax], [ohg])
                V(lambda e: e.tensor_tensor(out=gsh.t[:], in0=lg.t[:, :, 0:4], in1=R3(gmax.t[:], 4), op=ALU.subtract),
                  [lg, gmax], [gsh])
                A(lambda e: e.activation(out=gsh.t[:], in_=gsh.t[:], func=AF.Exp), [gsh], [gsh])
                V(lambda e: e.tensor_reduce(out=gsum.t[:], in_=gsh.t[:], axis=AX.X, op=ALU.add), [gsh], [gsum])
                V(lambda e: e.reciprocal(out=gp.t[:], in_=gsum.t[:]), [gsum], [gp])
                yield
                V(lambda e: e.tensor_tensor(out=tmp48.t[:], in0=lg.t[:, :, 4:36].rearrange("p j (g c) -> p j g c", g=4),
                                            in1=ohg.t[:].unsqueeze(3).to_broadcast([128, GT, 4, 8]), op=ALU.mult),
                  [lg, ohg], [tmp48])
                V(lambda e: e.tensor_reduce(out=esel.t[:], in_=tmp48.t[:].rearrange("p j g c -> p j c g"), axis=AX.X, op=ALU.add),
                  [tmp48], [esel])
                V(lambda e: e.tensor_reduce(out=m1.t[:], in_=esel.t[:], axis=AX.X, op=ALU.max), [esel], [m1])
                V(lambda e: e.tensor_tensor(out=oh1.t[:], in0=esel.t[:], in1=R3(m1.t[:], 8), op=ALU.is_equal), [esel, m1], [oh1])
                V(lambda e: e.scalar_tensor_tensor(out=em.t[:], in0=oh1.t[:], scalar=-1e30, in1=esel.t[:], op0=ALU.mult, op1=ALU.add),
                  [oh1, esel], [em])
                V(lambda e: e.tensor_reduce(out=m2.t[:], in_=em.t[:], axis=AX.X, op=ALU.max), [em], [m2])
                V(lambda e: e.tensor_tensor(out=oh2.t[:], in0=em.t[:], in1=R3(m2.t[:], 8), op=ALU.is_equal), [em, m2], [oh2])
                yield
                V(lambda e: e.tensor_tensor(out=d21.t[:], in0=m2.t[:], in1=m1.t[:], op=ALU.subtract), [m1, m2], [d21])
                A(lambda e: e.activation(out=e21.t[:], in_=d21.t[:], func=AF.Exp), [d21], [e21])
                V(lambda e: e.tensor_scalar(out=dn.t[:], in0=e21.t[:], scalar1=1.0, scalar2=None, op0=ALU.add), [e21], [dn])
                V(lambda e: e.reciprocal(out=dn.t[:], in_=dn.t[:]), [dn], [dn])
                V(lambda e: e.tensor_tensor(out=g1.t[:], in0=gp.t[:], in1=dn.t[:], op=ALU.mult), [gp, dn], [g1])
                V(lambda e: e.tensor_tensor(out=g2.t[:], in0=g1.t[:], in1=e21.t[:], op=ALU.mult), [g1, e21], [g2])
                yield
                for (ohx, ohxf) in ((oh1, oh1f), (oh2, oh2f)):
                    V(lambda e, ohx=ohx, ohxf=ohxf: e.tensor_tensor(
                        out=ohxf.t[:].rearrange("p j (g c) -> p j g c", g=4),
                        in0=ohg.t[:].unsqueeze(3).to_broadcast([128, GT, 4, 8]),
                        in1=ohx.t[:].unsqueeze(2).to_broadcast([128, GT, 4, 8]), op=ALU.mult), [ohg, ohx], [ohxf])
                V(lambda e: e.tensor_tensor(out=ohb.t[:], in0=oh1f.t[:], in1=oh2f.t[:], op=ALU.add), [oh1f, oh2f], [ohb])
                t0 = G * GT
                V(lambda e: e.tensor_copy(out=gates.t[:, t0:t0 + GT, 0], in_=g1.t[:]), [g1], [gates])
                V(lambda e: e.tensor_copy(out=gates.t[:, t0:t0 + GT, 1], in_=g2.t[:]), [g2], [gates])

            def stage_R2(G):
                par = G % 2
                oh1f, oh2f, ohb = oh1fs[par], oh2fs[par], ohbs[par]
                yield
                Prk = next_mm()
                for j in range(GT):
                    T(lambda e, j=j: e.matmul(Prk.t[:, j * 32:(j + 1) * 32], lhsT=Utri, rhs=ohb.t[:, j, :], start=True,
                                              stop=(j == 0)), [ohb, cbf], [Prk], inc=False)
                    for jj in range(j):
                        T(lambda e, j=j, jj=jj: e.matmul(Prk.t[:, j * 32:(j + 1) * 32], lhsT=ones_b, rhs=ohb.t[:, jj, :],
                                                         start=False, stop=(jj == j - 1)), [ohb, cbf], [Prk], inc=False)
                for j in range(GT):
                    T(lambda e, j=j: e.matmul(Prk.t[:, 256:288], lhsT=ones_b, rhs=ohb.t[:, j, :], start=(j == 0),
                                              stop=(j == GT - 1)), [ohb, cbf], [Prk], inc=(j == GT - 1))
                V(lambda e: e.tensor_tensor(out=rk.t[:], in0=Prk.t[:, 0:GT * 32].rearrange("p (j c) -> p j c", j=GT),
                                            in1=cnt.t[:].unsqueeze(1).to_broadcast([128, GT, 32]), op=ALU.add), [Prk, cnt], [rk])
                V(lambda e: e.tensor_tensor(out=cnt.t[:], in0=cnt.t[:], in1=Prk.t[:, 256:288], op=ALU.add), [Prk, cnt], [cnt])
                t0 = G * GT
                for ci_, ohxf in enumerate((oh1f, oh2f)):
                    V(lambda e, ohxf=ohxf: e.tensor_tensor(out=t32.t[:], in0=ohxf.t[:], in1=rk.t[:], op=ALU.mult), [ohxf, rk], [t32])
                    V(lambda e, ci_=ci_: e.tensor_reduce(out=rkf.t[:, t0:t0 + GT, ci_], in_=t32.t[:], axis=AX.X, op=ALU.add), [t32], [rkf])
                    V(lambda e, ohxf=ohxf: e.tensor_tensor(out=t32.t[:], in0=ohxf.t[:],
                                                           in1=iota_bc.unsqueeze(1).to_broadcast([128, GT, 32]), op=ALU.mult),
                      [ohxf, vbc], [t32])
                    V(lambda e, ci_=ci_: e.tensor_reduce(out=exf.t[:, t0:t0 + GT, ci_], in_=t32.t[:], axis=AX.X, op=ALU.add), [t32], [exf])
            NGr = NGrun if stop != 'p0' else 0
            if NGr < NG:
                zero_fill(NROWS // 128)
            for step in range(NGr + 4):
                gens = []
                if 0 <= step - 1 < NGr:
                    gens.append(stage_Ma(step - 1))
                    gens.append(stage_Mc(step - 1))
                if step < NGr:
                    gens.append(stage_F(step))
                if 0 <= step - 2 < NGr:
                    gens.append(stage_E(step - 2))
                if 0 <= step - 3 < NGr:
                    gens.append(stage_R1(step - 3))
                if 0 <= step - 4 < NGr:
                    gens.append(stage_R2(step - 4))
                while gens:
                    for g_ in list(gens):
                        try:
                            next(g_)
                        except StopIteration:
                            gens.remove(g_)
            S.barrier()

        with ExitStack() as esB:
          if stop is None or stop in ('B', 'C'):
              nbf = sb(esB, "nbf", [128, 32], F32)
              nbi = sb(esB, "nbi", [128, 32], I32)
              padded = sb(esB, "padded", [128, 32], F32)
              cumi = sb(esB, "cumi", [128, 32], F32)
              pstart = sb(esB, "pstart", [128, 32], F32)
              cmpb = sb(esB, "cmpb", [128, 64, 32], F32)
              bef = sb(esB, "bef", [128, 64], F32)
              CH = 16
              ohc = sb(esB, "ohc", [128, CH * 2, 32], F32)
              dsf = sb(esB, "dsf", [128, NT * 2], F32)
              hrow = [sb(esB, f"hrow{i}", [128, D], BF16) for i in range(8)]
              V(lambda e: e.tensor_scalar(out=nbf.t[:], in0=cnt.t[:], scalar1=1.0 / BLK,
                                          scalar2=float((BLK - 1) / BLK - 0.5 + 0.5 / BLK), op0=ALU.mult, op1=ALU.add),
                [cnt], [nbf])
              V(lambda e: e.tensor_copy(out=nbi.t[:], in_=nbf.t[:]), [nbf], [nbi])
              V(lambda e: e.tensor_copy(out=nbf.t[:], in_=nbi.t[:]), [nbi], [nbf])
              V(lambda e: e.tensor_scalar(out=padded.t[:], in0=nbf.t[:], scalar1=float(BLK), scalar2=None, op0=ALU.mult),
                [nbf], [padded])
              V(lambda e: e.tensor_copy(out=cumi.t[:], in_=padded.t[:]), [padded], [cumi])
              for e_ in range(1, NE):
                  V(lambda e, e_=e_: e.tensor_tensor(out=cumi.t[:, e_:e_ + 1], in0=cumi.t[:, e_ - 1:e_], in1=padded.t[:, e_:e_ + 1],
                                                     op=ALU.add), [cumi, padded], [cumi])
              V(lambda e: e.tensor_tensor(out=pstart.t[:], in0=cumi.t[:], in1=padded.t[:], op=ALU.subtract), [cumi, padded], [pstart])
              V(lambda e: e.tensor_tensor(out=cmpb.t[:], in0=cumi.t[:].unsqueeze(1).to_broadcast([128, 64, 32]),
                                          in1=bstart_bc.unsqueeze(2).to_broadcast([128, 64, 32]), op=ALU.is_le),
                [cumi, vbc], [cmpb])
              V(lambda e: e.tensor_reduce(out=bef.t[:], in_=cmpb.t[:], axis=AX.X, op=ALU.add), [cmpb], [bef])
              V(lambda e: e.tensor_scalar(out=bef.t[:], in0=bef.t[:], scalar1=float(NE - 1), scalar2=None, op0=ALU.min), [bef], [bef])
              V(lambda e: e.tensor_scalar(out=bef.t[:], in0=bef.t[:], scalar1=128.0, scalar2=vfm.t[:, 160:161],
                                          op0=ALU.mult, op1=ALU.add), [bef, vfm], [bef])
              unusedf = sb(esB, "unusedf", [128, 64], F32)
              nuf = sb(esB, "nuf", [128, 1], F32)
              V(lambda e: e.tensor_scalar(out=unusedf.t[:], in0=bstart_bc, scalar1=cumi.t[:, NE - 1:NE], scalar2=None,
                                          op0=ALU.is_ge), [cumi, vbc], [unusedf])
              V(lambda e: e.scalar_tensor_tensor(out=bef.t[:], in0=unusedf.t[:], scalar=16384.0, in1=bef.t[:], op0=ALU.mult,
                                                 op1=ALU.add), [unusedf, bef], [bef])
              V(lambda e: e.tensor_copy(out=widx.t[:], in_=bef.t[:]), [bef], [widx])
              V(lambda e: e.tensor_scalar(out=nuf.t[:], in0=cumi.t[:, NE - 1:NE], scalar1=1.0 / BLK, scalar2=None, op0=ALU.mult),
                [cumi], [nuf])
              V(lambda e: e.tensor_copy(out=nused_i.t[:], in_=nuf.t[:]), [nuf], [nused_i])
              exv = exf.t[:].rearrange("p t c -> p (t c)")
              rkv = rkf.t[:].rearrange("p t c -> p (t c)")
              for c0 in range(0, NT * 2, CH * 2):
                  n_ = min(CH * 2, NT * 2 - c0)
                  V(lambda e, c0=c0, n_=n_: e.tensor_tensor(out=ohc.t[:, 0:n_, :],
                                                            in0=iota_bc.unsqueeze(1).to_broadcast([128, n_, 32]),
                                                            in1=exv[:, c0:c0 + n_].unsqueeze(2).to_broadcast([128, n_, 32]),
                                                            op=ALU.is_equal), [exf, vbc], [ohc])
                  V(lambda e, n_=n_: e.tensor_tensor(out=ohc.t[:, 0:n_, :], in0=ohc.t[:, 0:n_, :],
                                                     in1=pstart.t[:].unsqueeze(1).to_broadcast([128, n_, 32]), op=ALU.mult),
                    [ohc, pstart], [ohc])
                  V(lambda e, c0=c0, n_=n_: e.tensor_reduce(out=dsf.t[:, c0:c0 + n_], in_=ohc.t[:, 0:n_, :], axis=AX.X, op=ALU.add),
                    [ohc], [dsf])
              V(lambda e: e.tensor_tensor(out=dsf.t[:], in0=dsf.t[:], in1=rkv, op=ALU.add), [dsf, rkf], [dsf])
              V(lambda e: e.tensor_copy(out=dest.t[:].rearrange("p t c -> p (t c)"), in_=dsf.t[:]), [dsf], [dest])
              if debug:
                  dbg = sb(esB, "dbgsb", [128, NT, 4], F32)
                  V(lambda e: e.tensor_copy(out=dbg.t[:, :, 0:2], in_=exf.t[:]), [exf], [dbg])
                  V(lambda e: e.tensor_copy(out=dbg.t[:, :, 2:4], in_=gates.t[:]), [gates, dbg], [dbg])
                  DM(lambda e: e.dma_start(out=dbg_d, in_=dbg.t[:]), [dbg], [])
              for t in range(NT):
                  hr = hrow[t % 8]
                  DM(lambda e, t=t, hr=hr: e.dma_start(out=hr.t[:], in_=hf_d[t * 128:(t + 1) * 128, :]), [hf_res[t]], [hr])
                  for ch in range(2):
                      DG(lambda e, t=t, ch=ch, hr=hr: e.indirect_dma_start(
                          out=xs_d[:, :], out_offset=bass.IndirectOffsetOnAxis(ap=dest.t[:, t, ch:ch + 1], axis=0),
                          in_=hr.t[:, :], in_offset=None, bounds_check=bc_reg, oob_is_err=False),
                         [dest, hr, xs_zero], [], multi=[xs_res])
              S.barrier()

        with ExitStack() as esC:
          if stop is None or stop == 'C':
              wg_bf = [sb(esC, f"wg{p}", [128, 8, DE], BF16) for p in range(2)]
              wu_bf = [sb(esC, f"wu{p}", [128, 8, DE], BF16) for p in range(2)]
              wd_bf = [sb(esC, f"wd{p}", [128, 4, D], BF16) for p in range(2)]
              wstg = [[sb(esC, f"wstg{q}{i}", [128, 4096], F32) for i in range(3)] for q in range(2)]
              xrow = [sb(esC, f"xrow{p}", [128, RT, D], BF16) for p in range(2)]
              xT = sb(esC, "xT", [128, 8, BLK], BF16)
              xT_res = [Res(f"xT{k}") for k in range(8)]
              hT = sb(esC, "hT", [128, 4, BLK], BF16)
              hT_res = [Res(f"hT{m}") for m in range(4)]
              sil = [sb(esC, f"sil{i}", [128, 512], F32) for i in range(2)]
              yo = [sb(esC, f"yo{i}", [128, D], BF16) for i in range(4)]
              tpc = [ps(esC, f"tpc{i}", [128, 1024], BF16) for i in range(2)]
              mc = [ps(esC, f"mc{i}", [128, 512], F32) for i in range(6)]
              stc = {"mm": 0, "tp": 0, "stg": 0, "cast": 0, "yo": 0}

              def nmm():
                  b = mc[stc["mm"] % 6]; stc["mm"] += 1; return b

              def ntp():
                  b = tpc[stc["tp"] % 2]; stc["tp"] += 1; return b

              def cast(out_ap, in_ap, rd, wr_multi):
                  eng = ("act", "act", "dve")[stc["cast"] % 3]
                  stc["cast"] += 1
                  if eng == "act":
                      A(lambda e: e.activation(out=out_ap, in_=in_ap, func=AF.Copy), rd, [], multi=wr_multi)
                  else:
                      S.op(eng, lambda e: e.tensor_copy(out=out_ap, in_=in_ap), rd, [], multi=wr_multi)

              wsrc = [wg_d.rearrange("e (q k) c -> (e q) (k c)", k=8),
                      wu_d.rearrange("e (q k) c -> (e q) (k c)", k=8),
                      wd_d.rearrange("e (q m) c -> (e q) (m c)", m=4)]

              def load_block(b):
                  p = b % 2
                  for i in range(3):
                      DG(lambda e, i=i: e.indirect_dma_start(
                          out=wstg[p][i].t[:, :], out_offset=None, in_=wsrc[i][:, :],
                          in_offset=bass.IndirectOffsetOnAxis(ap=widx.t[:, b:b + 1], axis=0),
                          bounds_check=bcw_reg, oob_is_err=False), [widx], [wstg[p][i]])

              def load_x_block(b):
                  p = b % 2
                  DM(lambda e: e.dma_start(out=xrow[p].t[:],
                                           in_=xs_d[b * BLK:(b + 1) * BLK, :].rearrange("(r p) c -> p r c", p=128)),
                     [xs_res], [xrow[p]])

              def cast_block(b):
                  p = b % 2
                  dsts = [wg_bf[p], wu_bf[p], wd_bf[p]]
                  for i in range(3):
                      dflat = dsts[i].t[:].rearrange("p a c -> p (a c)")
                      for pc in range(4):
                          cast(dflat[:, pc * 1024:(pc + 1) * 1024], wstg[p][i].t[:, pc * 1024:(pc + 1) * 1024], [wstg[p][i]], [dsts[i]])

              BMIN = (NTOK * 2 + BLK - 1) // BLK
              SKIP_ENGS = ("pe", "act", "dve", "sp")
              nu_reg = {}
              for q_ in SKIP_ENGS:
                  nu_reg[q_] = S.eng[q_].alloc_register("nused_" + q_)
                  S.wait_all(q_, [nused_i])
                  S.eng[q_].reg_load(nu_reg[q_], nused_i.t[0:1, 0:1])
              load_block(0)
              if NBLK > 1:
                  load_block(1)
              load_x_block(0)
              cast_block(0)
              for b in range(NBLK):
                  p = b % 2
                  if b + 2 < NBLK:
                      load_block(b + 2)
                  if b + 1 < NBLK:
                      load_x_block(b + 1)
                  ctx = None
                  if b >= BMIN and os.environ.get("K_NOSKIP") != "1":
                      snap = {k_: v_ for k_, v_ in S.waited.items() if k_[0] in SKIP_ENGS}
                      n0 = {q_: S.ccnt[q_] for q_ in SKIP_ENGS if q_ != "sp"}
                      ctx = True
                      S.defer = {q_: [] for q_ in SKIP_ENGS}
                      S.defer_dma = []
                  for k in range(8):
                      tp = ntp()
                      for r in range(RT):
                          T(lambda e, tp=tp, r=r, k=k: e.transpose(out=tp.t[:, r * 128:(r + 1) * 128],
                                                                    in_=xrow[p].t[:, r, :].rearrange("t (q k) -> t k q", k=8)[:, k, :],
                                                                    identity=ident), [xrow[p], cbf], [tp], inc=(r == RT - 1))
                      if k % 2 == 0:
                          V(lambda e, tp=tp, k=k: e.tensor_copy(out=xT.t[:, k, :], in_=tp.t[:, 0:BLK]), [tp], [], multi=[xT_res[k]])
                      else:
                          A(lambda e, tp=tp, k=k: e.activation(out=xT.t[:, k, :], in_=tp.t[:, 0:BLK], func=AF.Copy), [tp], [],
                            multi=[xT_res[k]])
                  if b + 1 < NBLK:
                      cast_block(b + 1)
                  for m in range(4):
                      Pg = nmm(); Pu = nmm()
                      for k in range(8):
                          T(lambda e, Pg=Pg, k=k, m=m: e.matmul(
                              Pg.t[:, :], lhsT=wg_bf[p].t[:, k, :].rearrange("p (j m) -> p m j", m=4)[:, m, :], rhs=xT.t[:, k, :],
                              start=(k == 0), stop=(k == 7)), [wg_bf[p], xT_res[k]], [Pg], inc=(k == 7))
                      for k in range(8):
                          T(lambda e, Pu=Pu, k=k, m=m: e.matmul(
                              Pu.t[:, :], lhsT=wu_bf[p].t[:, k, :].rearrange("p (j m) -> p m j", m=4)[:, m, :], rhs=xT.t[:, k, :],
                              start=(k == 0), stop=(k == 7)), [wu_bf[p], xT_res[k]], [Pu], inc=(k == 7))
                      sl = sil[m % 2]
                      A(lambda e, Pg=Pg, sl=sl: e.activation(out=sl.t[:], in_=Pg.t[:, :], func=AF.Silu), [Pg], [sl])
                      V(lambda e, Pu=Pu, sl=sl, m=m: e.tensor_tensor(out=hT.t[:, m, :], in0=Pu.t[:, :], in1=sl.t[:], op=ALU.mult),
                        [Pu, sl], [], multi=[hT_res[m]])
                  for r in range(RT):
                      Py = [nmm(), nmm()]
                      for hf_ in range(2):
                          for m in range(4):
                              T(lambda e, r=r, hf_=hf_, m=m, Py=Py: e.matmul(
                                  Py[hf_].t[:, :], lhsT=hT.t[:, m, r * 128:(r + 1) * 128],
                                  rhs=wd_bf[p].t[:, m, hf_ * 512:(hf_ + 1) * 512], start=(m == 0), stop=(m == 3)),
                                [hT_res[m], wd_bf[p]], [Py[hf_]], inc=(m == 3))
                      yo_ = yo[stc["yo"] % 4]; stc["yo"] += 1
                      A(lambda e, yo_=yo_, Py=Py: e.activation(out=yo_.t[:, 0:512], in_=Py[0].t[:, :], func=AF.Copy), [Py[0]], [], multi=[yo_])
                      V(lambda e, yo_=yo_, Py=Py: e.tensor_copy(out=yo_.t[:, 512:1024], in_=Py[1].t[:, :]), [Py[1]], [], multi=[yo_])
                      row0 = b * BLK + r * 128
                      DM(lambda e, yo_=yo_, row0=row0: e.dma_start(out=yb_d[row0:row0 + 128, :], in_=yo_.t[:]),
                         [yo_, yb_zero], [], multi=[yb_res])
                  if ctx is not None:
                      pend, S.defer = S.defer, None
                      with nc.sync.If_cmp(nu_reg["sp"], b, "IS_GT"):
                          for f_ in pend["sp"]:
                              f_()
                      with nc.sync.Else():
                          for (sem_, prev_) in S.defer_dma:
                              nc.sync.wait_ge(sem_, prev_)
                              nc.sync.sem_inc(sem_, 16)
                      for q_ in SKIP_ENGS:
                          if q_ == "sp":
                              continue
                          eng_ = S.eng[q_]
                          with eng_.If_cmp(nu_reg[q_], b, "IS_GT"):
                              for f_ in pend[q_]:
                                  f_()
                          n_inc = S.ccnt[q_] - n0[q_]
                          assert n0[q_] // ROT == (S.ccnt[q_] - 1) // ROT and n0[q_] > 0
                          sem_ = S.csem[q_][n0[q_] // ROT]
                          with eng_.Else():
                              eng_.wait_ge(sem_, n0[q_] - (n0[q_] // ROT) * ROT)
                              for i_ in range(n_inc):
                                  eng_.sem_inc(sem_, 1)
                      for k_ in [k_ for k_ in S.waited if k_[0] in SKIP_ENGS]:
                          if k_ in snap:
                              S.waited[k_] = snap[k_]
                          else:
                              del S.waited[k_]
              S.barrier()

        with ExitStack() as esD:
          if stop is None:
              y1 = [sb(esD, f"y1_{i}", [128, D], BF16) for i in range(4)]
              ob = [sb(esD, f"ob_{i}", [128, D], F32) for i in range(4)]
              y2 = [sb(esD, f"y2_{i}", [128, D], BF16) for i in range(4)]
              xr = [sb(esD, f"xr_{i}", [128, D], F32) for i in range(4)]
              ssd = [sb(esD, f"ssd{i}", [128, 1], F32) for i in range(4)]
              lnd = [sb(esD, f"lnd{i}", [128, 1], F32) for i in range(4)]
              rsd = [sb(esD, f"rsd{i}", [128, 1], F32) for i in range(4)]
              junkd = esD.enter_context(nc.sbuf_tensor("s_junkd", [128, 1024], BF16))
              out_res = Res("out")
              fnw = sb(esD, "fnw", [128, D], F32)
              DM(lambda e: e.dma_start(out=fnw.t[:], in_=fnw_d), [], [fnw])
              fnw_bc = fnw.t[:, :]
              ND, PF = 4, 3

              def fetch(t):
                  i = t % ND
                  DG(lambda e: e.indirect_dma_start(
                      out=y1[i].t[:, :], out_offset=None, in_=yb_d[:, :],
                      in_offset=bass.IndirectOffsetOnAxis(ap=dest.t[:, t, 0:1], axis=0),
                      bounds_check=bc_reg, oob_is_err=False), [dest, yb_res], [y1[i]])
                  DG(lambda e: e.indirect_dma_start(
                      out=y2[i].t[:, :], out_offset=None, in_=yb_d[:, :],
                      in_offset=bass.IndirectOffsetOnAxis(ap=dest.t[:, t, 1:2], axis=0),
                      bounds_check=bc_reg, oob_is_err=False), [dest, yb_res], [y2[i]])
                  DM(lambda e: e.dma_start(out=xr[i].t[:], in_=x1_d[t * 128:(t + 1) * 128, :]), [x1_res[t]], [xr[i]])

              def combine(t):
                  i = t % ND
                  V(lambda e: e.scalar_tensor_tensor(out=xr[i].t[:], in0=y1[i].t[:], scalar=gates.t[:, t, 0:1],
                                                     in1=xr[i].t[:], op0=ALU.mult, op1=ALU.add),
                    [y1[i], gates, xr[i]], [xr[i]])
                  V(lambda e: e.scalar_tensor_tensor(out=xr[i].t[:], in0=y2[i].t[:], scalar=gates.t[:, t, 1:2],
                                                     in1=xr[i].t[:], op0=ALU.mult, op1=ALU.add),
                    [y2[i], gates, xr[i]], [xr[i]])
                  A(lambda e: e.activation(out=junkd[:, :], in_=xr[i].t[:], func=AF.Square, accum_out=ssd[i].t[:]),
                    [xr[i]], [ssd[i]])
                  A(lambda e: e.activation(out=lnd[i].t[:], in_=ssd[i].t[:], func=AF.Ln, scale=1.0 / D, bias=EPS),
                    [ssd[i]], [lnd[i]])
                  A(lambda e: e.activation(out=rsd[i].t[:], in_=lnd[i].t[:], func=AF.Exp, scale=-0.5), [lnd[i]], [rsd[i]])
                  V(lambda e: e.scalar_tensor_tensor(out=ob[i].t[:], in0=xr[i].t[:], scalar=rsd[i].t[:, 0:1],
                                                     in1=fnw_bc, op0=ALU.mult, op1=ALU.mult),
                    [xr[i], rsd[i], fnw], [ob[i]])
                  DM(lambda e: e.dma_start(out=out_d[t * 128:(t + 1) * 128, :], in_=ob[i].t[:]),
                     [ob[i]], [], multi=[out_res])

              for t in range(min(PF, NT)):
                  fetch(t)
              for t in range(NT):
                  if t + PF < NT:
                      fetch(t + PF)
                  combine(t)
              S.wait_all("sp", [out_res])
        print(f"[build] instructions={S.n_ins} waits={S.n_wait}")
    return nc


def _consts():
    ident = np.eye(128, dtype=np.float32)
    tp = np.arange(128)
    U = (tp[:, None] < tp[None, :]).astype(np.float32)
    ones = np.ones((128, 128), np.float32)
    onesm = np.full((128, 128), 1.0 / 512, np.float32)
    maskcur = np.where(tp[None, :] >= tp[:, None], 0.0, NEG).astype(np.float32)
    maskprev = np.where(tp[None, :] < tp[:, None], 0.0, NEG).astype(np.float32)
    c = np.stack([ident, U, ones, onesm, maskcur, maskprev], axis=1)
    return c.reshape(128, 6 * 128).astype(ml_dtypes.bfloat16)


def _fm(v, nchunk):
    return np.ascontiguousarray(np.asarray(v, np.float32).reshape(nchunk, 128).T)


def prepare_inputs(inputs, nseq, n_cores):
    f = lambda k: np.asarray(inputs[k])
    x = f("x").astype(np.float32, copy=False)
    pos = f("positions")
    L = 0
    vfm = np.zeros((128, VF), np.float32)
    vfm[:, 0:8] = _fm(f("attn_norm_w")[L], 8)
    vfm[:, 8:16] = _fm(f("ffn_norm_w")[L], 8)
    vfm[:, 16:24] = _fm(np.concatenate([f("attn_out_norm_w")[L], f("conv_out_norm_w")[L]]), 8)
    vfm[:, 24:28] = _fm(f("conv_dw_b")[L], 4)
    vfm[:, 28:32] = _fm(f("conv_ln_w")[L], 4)
    vfm[:, 32:36] = _fm(f("conv_ln_b")[L], 4)
    dww = f("conv_dw_w")[L]
    vfm[:, 36:160] = dww.reshape(KTAPS, 4, 128).transpose(2, 1, 0).reshape(128, 4 * KTAPS)
    vfm[:, 160] = np.arange(128, dtype=np.float32)
    half = 8
    invf = np.power(np.float32(THETA), -np.arange(half, dtype=np.float32) * np.float32(2.0) / np.float32(16)).astype(np.float32)
    vbc1 = np.concatenate([f("ffn_norm_w")[L],
                           f("router_group_b")[L], f("router_expert_b")[L],
                           f("attn_sinks")[L], invf,
                           np.arange(NE).astype(np.float32),
                           (np.arange(64) * BLK).astype(np.float32)]).astype(np.float32)
    vbc = np.ascontiguousarray(np.broadcast_to(vbc1[None, :], (128, VB)))
    wr = np.ascontiguousarray(np.concatenate([f("router_group_w")[L], f("router_expert_w")[L]], axis=1))
    cbf = _consts()
    w_in0 = f("w_in")[L]
    qcols = np.concatenate([np.arange(h * 64, (h + 1) * 64) for h in (0, 4, 1, 5, 2, 6, 3, 7)])
    w_in_p = np.ascontiguousarray(np.concatenate([w_in0[:, qcols], w_in0[:, 512:]], axis=1))
    shared = {
        "w_in": w_in_p, "w_out": np.ascontiguousarray(f("w_out")[L]), "w_r": wr,
        "w_gate": np.ascontiguousarray(f("w_gate")[L]), "w_up": np.ascontiguousarray(f("w_up")[L]),
        "w_down": np.ascontiguousarray(f("w_down")[L]), "vfm": vfm, "vbc": vbc, "cbf": cbf,
        "fnw": np.ascontiguousarray(np.broadcast_to(f("final_norm_w").astype(np.float32)[None, :], (128, D))),
    }
    in_maps = []
    for c in range(n_cores):
        xs = x[c * nseq:(c + 1) * nseq].reshape(nseq * SEQ, D)
        p = pos[c * nseq:(c + 1) * nseq].reshape(nseq * NT_SEQ, 128).T
        m = dict(shared)
        m["x"] = np.ascontiguousarray(xs)
        m["pos"] = np.ascontiguousarray(p.astype(np.int32))
        in_maps.append(m)
    return in_maps


def kernel(**inputs):
    B = inputs["x"].shape[0]
    nseq = B // N_CORES
    nc = build_program(nseq)
    in_maps = prepare_inputs(inputs, nseq, N_CORES)
    res = run_bass_kernel_spmd(nc, in_maps, core_ids=list(range(N_CORES)))
    outs = [np.asarray(r["out"]).reshape(nseq, SEQ, D) for r in res.results]
    return np.concatenate(outs, axis=0).astype(np.float32, copy=False)
```
